# Optimizing a Trainium2 kernel written in Bass

```python
import jax, jax.numpy as jnp
from jax import lax
import numpy as np

D_MODEL = 1024
BATCH = 16
SEQ = 2048
DEPTH = 1

GRID_W = 64
HEAD_DIM = 64
RWKV_HEADS = 8
RWKV_WIDTH = RWKV_HEADS * HEAD_DIM
DECAY_RANK = 64
ICLR_RANK = 64
GATE_RANK = 128
NA_HEADS = 8
NA_WIDTH = NA_HEADS * HEAD_DIM
NA_MAX_WIN_ROWS = 8
NA_WIN_COLS = 16
N_GROUPS = 4
EXPERTS_PER_GROUP = 8
N_EXPERTS = N_GROUPS * EXPERTS_PER_GROUP
D_EXPERT = 256
TOP_K_IN_GROUP = 2
RMS_EPS = 1e-6
GN_EPS = 64e-5
NEG_INF = -1e30
RWKV_COLS = 3 * RWKV_WIDTH + 2 * DECAY_RANK + 2 * ICLR_RANK + GATE_RANK
NA_COLS = 3 * NA_WIDTH
GATE_COLS = 2 * D_MODEL
IN_COLS = RWKV_COLS + NA_COLS + GATE_COLS
RWKV_SPLITS = (RWKV_WIDTH, 2 * RWKV_WIDTH, 3 * RWKV_WIDTH,
               3 * RWKV_WIDTH + DECAY_RANK, 3 * RWKV_WIDTH + 2 * DECAY_RANK,
               3 * RWKV_WIDTH + 2 * DECAY_RANK + ICLR_RANK,
               3 * RWKV_WIDTH + 2 * DECAY_RANK + 2 * ICLR_RANK)
IN_SPLITS = (RWKV_COLS, RWKV_COLS + NA_WIDTH, RWKV_COLS + 2 * NA_WIDTH, RWKV_COLS + 3 * NA_WIDTH)

kernel_name = 'hybrid_rwkv7_natten2d_hiermoe_block'


def rmsnorm(x, g):
    x32 = x.astype(jnp.float32)
    y = x32 * lax.rsqrt(jnp.mean(x32 * x32, axis=-1, keepdims=True) + RMS_EPS)
    return (y * g).astype(x.dtype)


def centred_token_shift(p, mu_prev, mu_next):
    prev = jnp.pad(p, ((0, 0), (1, 0), (0, 0)))[:, :-1]
    nxt = jnp.pad(p, ((0, 0), (0, 1), (0, 0)))[:, 1:]
    return p + mu_prev * (prev - p) + mu_next * (nxt - p)


def rwkv_log_decay(lora, w0, up):
    w_raw = (w0 + jnp.tanh(lora) @ up).astype(jnp.float32)
    return -jnp.exp(-jax.nn.softplus(-w_raw) - 0.5)


def rwkv_step(S, inp):
    r_t, w_t, k_t, v_t, kk_t, a_t = inp
    sa = jnp.einsum('dbhij,dbhj->dbhi', S, -kk_t)
    S = (S * w_t[..., None, :] + sa[..., :, None] * (kk_t * a_t)[..., None, :]
         + v_t[..., :, None] * k_t[..., None, :])
    y = jnp.einsum('dbhij,dbhj->dbhi', S, r_t)
    return S, y


def rwkv7_bidirectional(p, mu_prev, mu_next, w0_f, w_up_f, w0_b, w_up_b, a0_f, a_up_f,
                        a0_b, a_up_b, g_up, k_k, k_a, r_k, lnx_g, lnx_b):
    B, T, _ = p.shape
    dt = p.dtype
    p = centred_token_shift(p, mu_prev, mu_next)
    r, k, v, wl_f, wl_b, al_f, al_b, gl = jnp.split(p, RWKV_SPLITS, axis=-1)
    lw_f = rwkv_log_decay(wl_f, w0_f, w_up_f)
    lw_b = rwkv_log_decay(wl_b, w0_b, w_up_b)
    a_f = jax.nn.sigmoid(a0_f + al_f @ a_up_f)
    a_b = jax.nn.sigmoid(a0_b + al_b @ a_up_b)
    g = jax.nn.sigmoid(gl) @ g_up

    def heads(t):
        return t.reshape(B, T, RWKV_HEADS, HEAD_DIM).astype(jnp.float32)

    r_h, v_h = heads(r), heads(v)
    kk = heads(k * k_k)
    kk = kk * lax.rsqrt(jnp.sum(kk * kk, axis=-1, keepdims=True) + 1e-12)
    k_f = heads(k * (1.0 + (a_f - 1.0) * k_a))
    k_b = heads(k * (1.0 + (a_b - 1.0) * k_a))
    a_fh, a_bh = heads(a_f), heads(a_b)
    w_fh, w_bh = jnp.exp(heads(lw_f)), jnp.exp(heads(lw_b))

    def both(f, b):
        return jnp.stack([f, jnp.flip(b, axis=1)], axis=0).transpose(2, 0, 1, 3, 4)

    xs = (both(r_h, r_h), both(w_fh, w_bh), both(k_f, k_b), both(v_h, v_h),
          both(kk, kk), both(a_fh, a_bh))
    S0 = jnp.zeros((2, B, RWKV_HEADS, HEAD_DIM, HEAD_DIM), jnp.float32)
    _, ys = lax.scan(rwkv_step, S0, xs)
    y_f = ys[:, 0].transpose(1, 0, 2, 3)
    y_b = jnp.flip(ys[:, 1].transpose(1, 0, 2, 3), axis=1)
    y = y_f + y_b
    mean = jnp.mean(y, axis=-1, keepdims=True)
    var = jnp.mean(jnp.square(y - mean), axis=-1, keepdims=True)
    y = ((y - mean) * lax.rsqrt(var + GN_EPS)).reshape(B, T, RWKV_WIDTH) * lnx_g + lnx_b
    bonus = jnp.sum(r_h * (k_f + k_b) * r_k, axis=-1, keepdims=True) * v_h
    y = (y + bonus.reshape(B, T, RWKV_WIDTH)) * g
    return y.astype(dt)


def neighbourhood_attention_2d(q, k, v, q_gain, k_gain, rpb):
    B, T, _ = q.shape
    dt = q.dtype
    rows = T // GRID_W
    kh = min(NA_MAX_WIN_ROWS, rows)

    def qk_norm(t, g):
        t32 = t.reshape(B, T, NA_HEADS, HEAD_DIM).astype(jnp.float32)
        return t32 * lax.rsqrt(jnp.mean(t32 * t32, axis=-1, keepdims=True) + RMS_EPS) * g

    def grid(t):
        return t.reshape(B, rows, GRID_W, NA_HEADS, HEAD_DIM).transpose(0, 3, 1, 2, 4)

    qg = grid(qk_norm(q, q_gain) * (HEAD_DIM ** -0.5))
    kg = grid(qk_norm(k, k_gain))
    vg = grid(v.reshape(B, T, NA_HEADS, HEAD_DIM).astype(jnp.float32))

    cols = np.arange(GRID_W)
    col_start = np.clip(cols - NA_WIN_COLS // 2, 0, GRID_W - NA_WIN_COLS)
    col_mask = ((cols[None, :] >= col_start[:, None])
                & (cols[None, :] < col_start[:, None] + NA_WIN_COLS))
    col_off = np.clip(cols[None, :] - cols[:, None] + NA_WIN_COLS - 1, 0, 2 * NA_WIN_COLS - 2)

    def row_block(i):
        start = jnp.clip(i - kh // 2, 0, rows - kh)
        k_blk = lax.dynamic_slice_in_dim(kg, start, kh, axis=2)
        v_blk = lax.dynamic_slice_in_dim(vg, start, kh, axis=2)
        q_blk = lax.dynamic_index_in_dim(qg, i, axis=2, keepdims=False)
        s = jnp.einsum('bhqd,bhrkd->bhqrk', q_blk, k_blk)
        row_off = start + jnp.arange(kh) - i + NA_MAX_WIN_ROWS - 1
        bias = rpb.astype(jnp.float32)[:, row_off][:, :, col_off]
        s = s + bias.transpose(0, 2, 1, 3)[None]
        s = jnp.where(col_mask[:, None, :], s, NEG_INF)
        prob = jax.nn.softmax(s, axis=(-2, -1))
        return jnp.einsum('bhqrk,bhrkd->bhqd', prob, v_blk)

    out = lax.map(row_block, jnp.arange(rows))
    out = out.transpose(1, 0, 3, 2, 4).reshape(B, T, NA_WIDTH)
    return out.astype(dt)


def hierarchical_moe(h, w_rg, b_rg, w_re, b_re, w_gate_e, w_up_e, w_down_e):
    B, T, D = h.shape
    ht = h.reshape(B * T, D)
    n_tok = ht.shape[0]
    g_logits = (ht @ w_rg + b_rg).astype(jnp.float32)
    g_sel = jnp.argmax(g_logits, axis=-1)
    p_group = jnp.take_along_axis(jax.nn.softmax(g_logits, axis=-1), g_sel[:, None], axis=-1)
    e_logits = (ht @ w_re + b_re).astype(jnp.float32).reshape(n_tok, N_GROUPS, EXPERTS_PER_GROUP)
    e_in_group = jnp.take_along_axis(e_logits, g_sel[:, None, None], axis=1)[:, 0]
    top_val, top_idx = lax.top_k(e_in_group, TOP_K_IN_GROUP)
    weights = p_group * jax.nn.softmax(top_val, axis=-1)
    expert_id = g_sel[:, None] * EXPERTS_PER_GROUP + top_idx
    combine = jnp.sum(jax.nn.one_hot(expert_id, N_EXPERTS, dtype=jnp.float32) * weights[..., None], axis=1)
    combine = combine.astype(h.dtype)
    y = jnp.zeros_like(ht)
    for e in range(N_EXPERTS):
        he = jax.nn.silu(ht @ w_gate_e[e]) * (ht @ w_up_e[e])
        y = y + combine[:, e:e + 1] * (he @ w_down_e[e])
    return y.reshape(B, T, D)


def setup_inputs(seed: int = 0) -> dict:
    key = jax.random.key(seed)
    ks = iter(jax.random.split(key, 40))
    L = DEPTH

    def nrm(shape, scale):
        return jax.random.normal(next(ks), shape, jnp.float32) * scale

    def unif(shape, lo, hi):
        return jax.random.uniform(next(ks), shape, jnp.float32, lo, hi)

    return {
        'x': nrm((BATCH, SEQ, D_MODEL), 1.0),
        'norm1_g': 1.0 + nrm((L, D_MODEL), 0.02),
        'w_in': nrm((L, D_MODEL, IN_COLS), D_MODEL ** -0.5),
        'mu_prev': unif((L, RWKV_COLS), 0.0, 0.5),
        'mu_next': unif((L, RWKV_COLS), 0.0, 0.5),
        'w0_f': unif((L, RWKV_WIDTH), -6.0, 1.0),
        'w_up_f': nrm((L, DECAY_RANK, RWKV_WIDTH), DECAY_RANK ** -0.5),
        'w0_b': unif((L, RWKV_WIDTH), -6.0, 1.0),
        'w_up_b': nrm((L, DECAY_RANK, RWKV_WIDTH), DECAY_RANK ** -0.5),
        'a0_f': nrm((L, RWKV_WIDTH), 0.5),
        'a_up_f': nrm((L, ICLR_RANK, RWKV_WIDTH), ICLR_RANK ** -0.5),
        'a0_b': nrm((L, RWKV_WIDTH), 0.5),
        'a_up_b': nrm((L, ICLR_RANK, RWKV_WIDTH), ICLR_RANK ** -0.5),
        'g_up': nrm((L, GATE_RANK, RWKV_WIDTH), GATE_RANK ** -0.5),
        'k_k': 0.85 + nrm((L, RWKV_WIDTH), 0.05),
        'k_a': 1.0 + nrm((L, RWKV_WIDTH), 0.05),
        'r_k': nrm((L, RWKV_HEADS, HEAD_DIM), 0.1),
        'lnx_g': 1.0 + nrm((L, RWKV_WIDTH), 0.02),
        'lnx_b': nrm((L, RWKV_WIDTH), 0.01),
        'q_gain': 1.0 + nrm((L, HEAD_DIM), 0.02),
        'k_gain': 1.0 + nrm((L, HEAD_DIM), 0.02),
        'rpb': nrm((L, NA_HEADS, 2 * NA_MAX_WIN_ROWS - 1, 2 * NA_WIN_COLS - 1), 0.1),
        'b_gate': nrm((L, GATE_COLS), 0.01),
        'w_o_rwkv': nrm((L, RWKV_WIDTH, D_MODEL), RWKV_WIDTH ** -0.5),
        'w_o_na': nrm((L, NA_WIDTH, D_MODEL), NA_WIDTH ** -0.5),
        'w_out': nrm((L, D_MODEL, D_MODEL), D_MODEL ** -0.5),
        'norm2_g': 1.0 + nrm((L, D_MODEL), 0.02),
        'w_router_group': nrm((L, D_MODEL, N_GROUPS), D_MODEL ** -0.5),
        'b_router_group': nrm((L, N_GROUPS), 0.01),
        'w_router_expert': nrm((L, D_MODEL, N_EXPERTS), D_MODEL ** -0.5),
        'b_router_expert': nrm((L, N_EXPERTS), 0.01),
        'w_gate_e': nrm((L, N_EXPERTS, D_MODEL, D_EXPERT), D_MODEL ** -0.5),
        'w_up_e': nrm((L, N_EXPERTS, D_MODEL, D_EXPERT), D_MODEL ** -0.5),
        'w_down_e': nrm((L, N_EXPERTS, D_EXPERT, D_MODEL), D_EXPERT ** -0.5),
    }


def reference(x, norm1_g, w_in, mu_prev, mu_next, w0_f, w_up_f, w0_b, w_up_b, a0_f, a_up_f,
              a0_b, a_up_b, g_up, k_k, k_a, r_k, lnx_g, lnx_b, q_gain, k_gain, rpb, b_gate,
              w_o_rwkv, w_o_na, w_out, norm2_g, w_router_group, b_router_group,
              w_router_expert, b_router_expert, w_gate_e, w_up_e, w_down_e):
    for l in range(DEPTH):
        h = rmsnorm(x, norm1_g[l])
        p = h @ w_in[l]
        p_rwkv, q, k, v, gate_logits = jnp.split(p, IN_SPLITS, axis=-1)
        y_a = rwkv7_bidirectional(p_rwkv, mu_prev[l], mu_next[l], w0_f[l], w_up_f[l], w0_b[l],
                                  w_up_b[l], a0_f[l], a_up_f[l], a0_b[l], a_up_b[l], g_up[l],
                                  k_k[l], k_a[l], r_k[l], lnx_g[l], lnx_b[l]) @ w_o_rwkv[l]
        y_b = neighbourhood_attention_2d(q, k, v, q_gain[l], k_gain[l], rpb[l]) @ w_o_na[l]
        g_a, g_b = jnp.split(jax.nn.sigmoid(gate_logits + b_gate[l]), 2, axis=-1)
        x = x + (g_a * y_a + g_b * y_b) @ w_out[l]
        x = x + hierarchical_moe(rmsnorm(x, norm2_g[l]), w_router_group[l], b_router_group[l],
                                 w_router_expert[l], b_router_expert[l], w_gate_e[l],
                                 w_up_e[l], w_down_e[l])
    return x
```

```python
import math
import numpy as np
from contextlib import ExitStack
import concourse.bass as bass
import concourse.mybir as mybir
from concourse.bass_utils import run_bass_kernel_spmd

F32 = mybir.dt.float32
BF16 = mybir.dt.bfloat16
AF = mybir.ActivationFunctionType
ALU = mybir.AluOpType
AX = mybir.AxisListType

NCORES = 8
T = 2048
NT = 4096
D = 1024
KAPPA = math.exp(-0.5)
NEG = -30000.0


class Res:
    __slots__ = ("name", "writers", "readers", "excl")

    def __init__(self, name="", excl=False):
        self.name = name
        self.writers = []
        self.readers = []
        self.excl = excl


class _PEProxy:
    def __init__(self, sched):
        self.s = sched

    def matmul(self, out, lhsT, rhs, **kw):
        s = self.s
        shp = list(lhsT.shape)
        k = shp[0]
        m = 1
        for d in shp[1:]:
            m *= d
        rnd = lambda v: 32 if v <= 32 else (64 if v <= 64 else 128)
        mode = (rnd(k), rnd(m), str(lhsT.dtype), lhsT.base_partition())
        if s.pe_mode is not None and mode != s.pe_mode and s.last_tok["pe"] is not None:
            s._emit_waits("pe", [s.last_tok["pe"]])
        s.pe_mode = mode
        return s.nc.tensor.matmul(out, lhsT, rhs, **kw)


class Sched:
    EPOCH = 6000

    def __init__(self, nc, stack, n_dma_sems=24, needed=None):
        self.nc = nc
        self.stack = stack
        self.needed_in = needed
        self.needed_out = set()
        self.incmap = {}
        self.inc_count = {}
        self.eng = {"pe": nc.tensor, "act": nc.scalar, "dve": nc.vector,
                    "pool": nc.gpsimd, "sp": nc.sync}
        self.cur_sem = {}
        self.cnt = {}
        self.seen = {e: {} for e in self.eng}
        self.sems = {}
        self.nsem = 0
        for e in self.eng:
            self._new_epoch(e)
        self.dma_keys = []
        self.dma_cnt = []
        for i in range(n_dma_sems):
            k = ("dma", i)
            self.sems[k] = stack.enter_context(nc.semaphore(f"dq{i}"))
            self.dma_keys.append(k)
            self.dma_cnt.append(0)
        self.dma_rr = 0
        self.dma_rr_sw = 0
        self.last_tok = {e: None for e in self.eng}
        self.all_dma_toks = {}
        self.n_inst = {e: 0 for e in self.eng}
        self.n_wait = 0
        self.pe_mode = None
        self.pe_proxy = _PEProxy(self)

    def _new_epoch(self, e):
        k = (e, self.nsem)
        self.nsem += 1
        self.sems[k] = self.stack.enter_context(self.nc.semaphore(f"e_{e}_{self.nsem}"))
        self.cur_sem[e] = k
        self.cnt[e] = 0

    def _emit_waits(self, e, deps):
        need = {}
        for tok in deps:
            if tok is None:
                continue
            k, v = tok
            if self.seen[e].get(k, 0) >= v:
                continue
            if need.get(k, 0) < v:
                need[k] = v
        eng = self.eng[e]
        for k, v in need.items():
            if k[0] == "dma":
                val = v
            else:
                self.needed_out.add((k, v))
                val = self.incmap[(k, v)] if self.needed_in is not None else v
            eng.wait_ge(self.sems[k], val)
            self.seen[e][k] = v
            self.n_wait += 1

    def _deps(self, reads, writes, e=None):
        deps = []
        other = lambda t: t[0][0] != e
        for r in reads:
            if e == "pe":
                deps.extend(t for t in r.writers if other(t))
            else:
                deps.extend(r.writers)
            if r.excl:
                deps.extend(t for t in r.readers if other(t))
        for w in writes:
            deps.extend(t for t in w.writers if other(t))
            deps.extend(t for t in w.readers if other(t))
        return deps

    def _commit(self, tok, reads, writes, add=False):
        for r in reads:
            r.readers.append(tok)
            if len(r.readers) > 48:
                best = {}
                for k, v in r.readers:
                    if best.get(k, 0) < v:
                        best[k] = v
                r.readers = list(best.items())
        for w in writes:
            if add:
                w.writers.append(tok)
            else:
                w.writers = [tok]
            w.readers = []

    def op(self, e, fn, reads=(), writes=(), add=False):
        if self.cnt[e] >= self.EPOCH:
            self._new_epoch(e)
        self._emit_waits(e, self._deps(reads, writes, e))
        ins = fn(self.pe_proxy if e == "pe" else self.eng[e])
        k = self.cur_sem[e]
        self.cnt[e] += 1
        tok = (k, self.cnt[e])
        if self.needed_in is None:
            ins.then_inc(self.sems[k], 1)
        elif tok in self.needed_in:
            ins.then_inc(self.sems[k], 1)
            self.inc_count[k] = self.inc_count.get(k, 0) + 1
            self.incmap[tok] = self.inc_count[k]
            self.n_inc = getattr(self, "n_inc", 0) + 1
        self.last_tok[e] = tok
        self.n_inst[e] += 1
        self._commit(tok, reads, writes, add)
        return tok

    def dma(self, e, out, in_, reads=(), writes=(), add=False, **kw):
        nsw = len(self.dma_keys) // 4
        if e == "pool":
            i = self.dma_rr_sw
            self.dma_rr_sw = (self.dma_rr_sw + 1) % nsw
        else:
            i = nsw + self.dma_rr
            self.dma_rr = (self.dma_rr + 1) % (len(self.dma_keys) - nsw)
        k = self.dma_keys[i]
        deps = self._deps(reads, writes, e)
        if self.dma_cnt[i] > 0:
            deps.append((k, 16 * self.dma_cnt[i]))
        self._emit_waits(e, deps)
        ins = self.eng[e].dma_start(out=out, in_=in_, **kw)
        ins.then_inc(self.sems[k], 16)
        self.dma_cnt[i] += 1
        tok = (k, 16 * self.dma_cnt[i])
        self.all_dma_toks[k] = tok
        self.n_inst[e] += 1
        self._commit(tok, reads, writes, add)
        return tok

    def barrier(self):
        toks = [t for t in self.last_tok.values() if t is not None]
        toks += list(self.all_dma_toks.values())
        for e in self.eng:
            self._emit_waits(e, toks)


def build(debug=False, phases="ABCDE", needed=None, info=None):
    nc = bass.Bass("TRN2", target_bir_lowering=False)

    def din(name, shape, dt=F32):
        return nc.dram_tensor(name, list(shape), dt, kind="ExternalInput").ap()

    skind = "ExternalOutput" if debug else "Internal"

    def dscr(name, shape, dt):
        return nc.dram_tensor(name, list(shape), dt, kind=skind).ap()

    x = din("x", [NT, D])
    g1 = din("g1", [1, D])
    g2 = din("g2", [1, D])
    w_in = din("w_in", [D, 5504])
    mus = din("mus", [128, 2, 15])
    vec = din("vec", [128, 9, 4])
    lora = din("lora", [128, 3, 512])
    qkg = din("qkg", [128, 2])
    btab = din("btab", [128, 8, 14, 64])
    nmask = din("nmask", [128, 64])
    bg = din("bg", [128, 16])
    w_oa = din("w_oa", [512, D])
    w_ob = din("w_ob", [512, D])
    w_out = din("w_out", [D, D])
    wr = din("wr", [D, 36])
    br = din("br", [1, 36])
    wge = din("wge", [32, D, 256])
    wue = din("wue", [32, D, 256])
    wde = din("wde", [32, 256, D])
    identd = din("ident", [128, 128])
    bonesd = din("bones", [128, 128])
    rmaskd = din("rmask", [128, 4, 2, 64])
    scand = din("scanm", [128, T])
    out = nc.dram_tensor("out", [NT, D], F32, kind="ExternalOutput").ap()

    hT_d = dscr("hT_d", [D, NT], BF16)
    p_d = dscr("p_d", [3456, NT], F32)
    ya_d = din("ya_d", [512, NT], BF16) if (debug and "B" not in phases) else dscr("ya_d", [512, NT], BF16)
    yb_d = dscr("yb_d", [512, NT], BF16)
    wgu_s = nc.dram_tensor("wgu_s", [32, 128, 8, 512], BF16, kind="Internal").ap()
    wdn_s = nc.dram_tensor("wdn_s", [32, 128, 2, D], BF16, kind="Internal").ap()
    xm_d = din("xm_d", [NT, D], F32) if (debug and "D" not in phases) else dscr("xm_d", [NT, D], F32)

    with ExitStack() as gst:
        S = Sched(nc, gst, n_dma_sems=32, needed=needed)
        PB = [gst.enter_context(nc.psum_tensor(f"pb{i}", [128, 512], F32)) for i in range(8)]
        RP = [Res(f"pb{i}", excl=True) for i in range(8)]

        def gsb(n, s, d):
            return gst.enter_context(nc.sbuf_tensor(n, s, d))

        identf = gsb("identf", [128, 128], F32); r_idf = Res()
        identb = gsb("identb", [128, 128], BF16); r_idb = Res()
        S.dma("sp", identf[:], identd, writes=[r_idf])
        S.op("dve", lambda e: e.tensor_copy(identb[:], identf[:]), reads=[r_idf], writes=[r_idb])

        rr = {"ev": 0}

        conv_jobs = []
        for e_ in range(32):
            conv_jobs.append((wgu_s[e_][:, :, 0:256], wge[e_].rearrange("(k p) n -> p k n", p=128)))
            conv_jobs.append((wgu_s[e_][:, :, 256:512], wue[e_].rearrange("(k p) n -> p k n", p=128)))
            conv_jobs.append((wdn_s[e_], wde[e_].rearrange("(c p) n -> p c n", p=128)))
        conv_jobs.reverse()

        def conv_step(n=1):
            while n > 0 and conv_jobs:
                o_, i_ = conv_jobs.pop()
                S.dma("pool", o_, i_)
                n -= 1

        def evac(out_ap, in_ap, reads, writes, add=False):
            rr["ev"] += 1
            if rr["ev"] % 2:
                return S.op("act", lambda e: e.copy(out_ap, in_ap), reads=reads, writes=writes, add=add)
            return S.op("dve", lambda e: e.tensor_copy(out_ap, in_ap), reads=reads, writes=writes, add=add)

        if "A" in phases:
            with ExitStack() as st:
                def sb(n, s, d):
                    return st.enter_context(nc.sbuf_tensor(n, s, d))
                wA = sb("wA", [128, 8, 3456], BF16); r_wA = Res()
                for k in range(8):
                    S.dma("pool", wA[:, k, :], w_in[k * 128:(k + 1) * 128, 0:3456], writes=[r_wA], add=True)
                g1t = sb("g1t", [128, D], F32); r_g1 = Res()
                S.dma("sp", g1t[:], g1.broadcast_to([128, D]), writes=[r_g1])
                xt = [sb(f"xt{i}", [128, 4, D], F32) for i in range(2)]; r_xt = [Res(), Res()]
                junk = sb("junk", [128, D], F32)
                ss = [sb(f"ss{i}", [128, 4], F32) for i in range(2)]; r_ss = [Res(), Res()]
                hb = sb("hb", [128, 4, D], BF16); r_hb = Res()
                hT = [sb(f"hT{i}", [128, 8, 512], BF16) for i in range(2)]; r_hT = [Res(), Res()]
                stg = [sb(f"stg{i}", [128, 512], F32) for i in range(4)]; r_stg = [Res() for _ in range(4)]
                for tb in range(8):
                    i = tb % 2
                    S.dma("sp", xt[i][:], x[tb * 512:(tb + 1) * 512, :].rearrange("(j p) d -> p j d", p=128),
                          writes=[r_xt[i]])
                    for j in range(4):
                        S.op("act", lambda e, j=j: e.activation(junk[:], xt[i][:, j, :], AF.Square,
                                                               accum_out=ss[i][:, j:j + 1]),
                             reads=[r_xt[i]], writes=[r_ss[i]], add=(j > 0))
                    S.op("act", lambda e: e.activation(ss[i][:], ss[i][:], AF.Sqrt, bias=1e-6, scale=1.0 / D),
                         reads=[r_ss[i]], writes=[r_ss[i]])
                    S.op("dve", lambda e: e.reciprocal(ss[i][:], ss[i][:]), reads=[r_ss[i]], writes=[r_ss[i]])
                    for j in range(4):
                        S.op("dve", lambda e, j=j: e.scalar_tensor_tensor(hb[:, j, :], xt[i][:, j, :], ss[i][:, j:j + 1],
                                                                         g1t[:], ALU.mult, ALU.mult),
                             reads=[r_xt[i], r_ss[i], r_g1], writes=[r_hb], add=(j > 0))
                    for k in range(8):
                        b = k % 4
                        for j in range(4):
                            S.op("pe", lambda e, j=j, k=k, b=b: e.matmul(PB[b][:, j * 128:(j + 1) * 128],
                                                                       hb[:, j, k * 128:(k + 1) * 128], identb[:],
                                                                       start=True, stop=True),
                                 reads=[r_hb, r_idb], writes=[RP[b]], add=(j > 0))
                        evac(hT[i][:, k, :], PB[b][:], [RP[b]], [r_hT[i]], add=(k > 0))
                    S.dma("pool", hT_d.rearrange("(k p) t -> p k t", p=128)[:, :, tb * 512:(tb + 1) * 512], hT[i][:],
                          reads=[r_hT[i]])
                    for cc in range(27):
                        b = 4 + cc % 4
                        for k in range(8):
                            S.op("pe", lambda e, k=k, cc=cc, b=b: e.matmul(PB[b][:], wA[:, k, cc * 128:(cc + 1) * 128],
                                                                         hT[i][:, k, :], start=(k == 0), stop=(k == 7)),
                                 reads=[r_wA, r_hT[i]], writes=[RP[b]], add=(k > 0))
                        q = cc % 4
                        evac(stg[q][:], PB[b][:], [RP[b]], [r_stg[q]])
                        S.dma("pool", p_d[cc * 128:(cc + 1) * 128, tb * 512:(tb + 1) * 512], stg[q][:], reads=[r_stg[q]])
                S.barrier()

        PHASE_B(nc, S, PB, RP, locals()) if "B" in phases else None
        conv_step(1000)
        PHASE_C(nc, S, PB, RP, locals()) if "C" in phases else None
        PHASE_D(nc, S, PB, RP, locals()) if "D" in phases else None
        PHASE_E(nc, S, PB, RP, locals()) if "E" in phases else None
        S.barrier()
        print("inst", S.n_inst, "waits", S.n_wait, "sems", S.nsem, "incs", getattr(S, "n_inc", None))
        if info is not None:
            info["needed"] = S.needed_out
    return nc


def build2(debug=False, phases="ABCDE"):
    info = {}
    build(debug=debug, phases=phases, info=info)
    return build(debug=debug, phases=phases, needed=info["needed"])


def PHASE_B(nc, S, PB, RP, G):
    p_d, ya_d, mus, vec, lora, bonesd, rmaskd, scand = (G[k] for k in ("p_d", "ya_d", "mus", "vec", "lora", "bonesd", "rmaskd", "scand"))
    identb, r_idb, identf, r_idf = G["identb"], G["r_idb"], G["identf"], G["r_idf"]
    GN_EPS = 64e-5
    with ExitStack() as st:
        def sb(n, s, d):
            return st.enter_context(nc.sbuf_tensor(n, s, d))
        mu = sb("mu", [128, 2, 15], F32); r_mu = Res()
        c0 = sb("c0", [128, 15], F32)
        vc = sb("vc", [128, 9, 4], F32); r_vc = Res()
        omka = sb("omka", [128, 4], F32)
        lwt = sb("lwt", [128, 3, 512], BF16); r_lwt = Res()
        bones = sb("bonesB", [128, 128], F32); r_bones = Res()
        RM = sb("RM", [128, 4, 2, 64], F32); r_RM = Res()
        scm = sb("scm", [128, T], BF16); r_scm = Res()
        S.dma("sp", mu[:], mus, writes=[r_mu])
        S.dma("sp", vc[:], vec, writes=[r_vc])
        S.dma("pool", lwt[:], lora, writes=[r_lwt])
        S.dma("sp", bones[:], bonesd, writes=[r_bones])
        S.dma("sp", RM[:], rmaskd, writes=[r_RM])
        S.dma("pool", scm[:], scand, writes=[r_scm])
        S.op("dve", lambda e: e.tensor_tensor(c0[:], mu[:, 0, :], mu[:, 1, :], ALU.add), reads=[r_mu], writes=[r_mu])
        S.op("dve", lambda e: e.tensor_scalar(c0[:], c0[:], -1.0, 1.0, ALU.mult, ALU.add), reads=[r_mu], writes=[r_mu])
        S.op("dve", lambda e: e.tensor_scalar(omka[:], vc[:, 5, :], -1.0, 1.0, ALU.mult, ALU.add), reads=[r_vc], writes=[r_vc])
        Pt = sb("Pt", [128, T + 2], F32); r_Pt = Res()
        S.op("pool", lambda e: e.memset(Pt[:], 0.0), writes=[r_Pt])
        Pt2 = sb("Pt2", [128, T + 2], F32); r_Pt2 = Res()
        S.op("pool", lambda e: e.memset(Pt2[:], 0.0), writes=[r_Pt2])
        slc = {"n": 0}
        L = [sb(f"L{i}", [128, T], BF16) for i in range(3)]; r_L = [Res() for _ in range(3)]
        names = ["r_s", "k_s", "v_s", "a_f", "a_b", "sg", "kkn", "t1", "t2", "bonus"]
        Tt = {n: sb("B_" + n, [128, T], F32) for n in names}
        Rt = {n: Res(n) for n in names}
        HAT = [sb(f"HAT{d}", [128, 32, 4, 64], BF16) for d in range(2)]; r_HAT = [Res(), Res()]
        ytm = sb("ytm", [128, 32, 64], F32); r_ytm = Res()
        yo = sb("yo", [128, T], BF16); r_yo = Res()
        vb = sb("vbB", [128, T], BF16); r_vb = Res()
        PCs = [sb(f"PCs{d}", [128, 32], F32) for d in range(2)]; r_PCs = [Res(), Res()]
        totb = sb("totb", [128, 32], F32); r_totb = Res()
        ytmp = sb("ytmp", [128, 128], F32); r_ytmp = Res()
        gs = sb("gs", [128, 4, 32], F32); r_gs = Res()

        def ztile(n, shp, dt):
            t_ = sb(n, shp, dt)
            S.op("pool", lambda e: e.memset(t_[:], 0.0))
            return t_
        Nbd = [ztile(f"Nbd{i}", [128, 2, 128], BF16) for i in range(2)]; r_Nbd = [Res(), Res()]
        Abd = [ztile(f"Abd{i}", [128, 2, 128], BF16) for i in range(2)]; r_Abd = [Res(), Res()]
        Akabd = [ztile(f"Akabd{i}", [128, 2, 128], BF16) for i in range(2)]; r_Akabd = [Res(), Res()]
        ATbd = [ztile(f"ATbd{i}", [128, 2, 128], BF16) for i in range(2)]; r_ATbd = [Res(), Res()]
        KD = [ztile(f"KD{i}", [128, 2, 128], BF16) for i in range(2)]; r_KD = [Res(), Res()]
        Nst = [ztile(f"Nst{i}", [128, 2, 128], BF16) for i in range(2)]; r_Nst = [Res(), Res()]
        Ast = [ztile(f"Ast{i}", [128, 2, 128], BF16) for i in range(2)]; r_Ast = [Res(), Res()]
        Xbs = [sb(f"Xb{i}", [128, 2, 128], BF16) for i in range(2)]; r_Xbs = [Res(), Res()]
        T0 = sb("T0", [128, 256], BF16); r_T0 = Res()
        T1 = sb("T1", [128, 256], BF16); r_T1 = Res()
        Vst = [sb(f"Vst{i}", [128, 2, 64], BF16) for i in range(2)]; r_Vst = [Res(), Res()]
        QM1bd = ztile("QM1bd", [128, 2, 2, 128], BF16); r_QM1bd = Res()
        QM2bd = ztile("QM2bd", [128, 2, 2, 128], BF16); r_QM2bd = Res()
        STs = sb("STs", [128, 2, 64], BF16); r_STs = Res()
        Icst = sb("Icst", [128, 2, 2, 64], F32); I2b = sb("I2b", [128, 64], BF16); MQ2 = sb("MQ2", [128, 2, 2, 64], F32); r_cst = Res()
        S.op("pool", lambda e: e.memset(Icst[:], 0.0), writes=[r_cst])
        for d_ in range(2):
            S.op("dve", lambda e: e.tensor_tensor(Icst[:, d_, 1, :], identf[:, 0:64], identf[:, 64:128], ALU.add), reads=[r_idf, r_cst], writes=[r_cst])
        S.op("dve", lambda e: e.tensor_copy(I2b[:], Icst[:, 0, 1, :]), reads=[r_cst], writes=[r_cst])
        S.op("dve", lambda e: e.tensor_copy(MQ2[:, :, 0, :], RM[:, 1]), reads=[r_RM, r_cst], writes=[r_cst])
        S.op("dve", lambda e: e.tensor_copy(MQ2[:, :, 1, :], RM[:, 3]), reads=[r_RM, r_cst], writes=[r_cst])
        S.barrier()

        def v3(ap):
            return ap.rearrange("p (c t) -> p c t", t=64)

        def shift_load(fc, s_, dst, r_dst):
            slc["n"] += 1
            Pb, r_Pb = (Pt, r_Pt) if slc["n"] % 2 else (Pt2, r_Pt2)
            S.dma("sp", Pb[:, 1:T + 1], p_d[fc * 128:(fc + 1) * 128, s_ * T:(s_ + 1) * T], writes=[r_Pb])
            S.op("act", lambda e: e.mul(dst[:], Pb[:, 1:T + 1], c0[:, fc:fc + 1]), reads=[r_Pb, r_mu], writes=[r_dst])
            S.op("dve", lambda e: e.scalar_tensor_tensor(dst[:], Pb[:, 0:T], mu[:, 0, fc:fc + 1], dst[:], ALU.mult, ALU.add),
                 reads=[r_Pb, r_mu, r_dst], writes=[r_dst])
            S.op("dve", lambda e: e.scalar_tensor_tensor(dst[:], Pb[:, 2:T + 2], mu[:, 1, fc:fc + 1], dst[:], ALU.mult, ALU.add),
                 reads=[r_Pb, r_mu, r_dst], writes=[r_dst])

        def mm_sig(dst, r_dst, li, rows, bias_ap, hp, func):
            for c in range(4):
                b = c % 2
                S.op("pe", lambda e: e.matmul(PB[b][:], lwt[rows, li, hp * 128:(hp + 1) * 128], L[li][rows, c * 512:(c + 1) * 512],
                                              start=True, stop=True), reads=[r_lwt, r_L[li]], writes=[RP[b]])
                if func is None:
                    S.op("act", lambda e: e.copy(dst[:, c * 512:(c + 1) * 512], PB[b][:]), reads=[RP[b]], writes=[r_dst], add=(c > 0))
                else:
                    S.op("act", lambda e: e.activation(dst[:, c * 512:(c + 1) * 512], PB[b][:], func, bias=bias_ap),
                         reads=[RP[b], r_vc], writes=[r_dst], add=(c > 0))

        def bones_mm(src, r_src, post):
            for c in range(4):
                b = c % 2
                S.op("pe", lambda e: e.matmul(PB[b][:], bones[:], src[:, c * 512:(c + 1) * 512], start=True, stop=True),
                     reads=[r_bones, r_src], writes=[RP[b]])
                post(c, b)

        r_s, k_s, v_s, a_f, a_b, sg, kkn, t1, t2, bonus = (Tt[n] for n in names)
        X = Pt[:, 1:T + 1]
        for s_ in range(2):
            shift_load(12, s_, t1, Rt["t1"])
            S.op("act", lambda e: e.activation(L[0][:], t1[:], AF.Tanh), reads=[Rt["t1"]], writes=[r_L[0]])
            shift_load(13, s_, t1, Rt["t1"])
            S.op("act", lambda e: e.copy(L[1][:], t1[:]), reads=[Rt["t1"]], writes=[r_L[1]])
            shift_load(14, s_, t1, Rt["t1"])
            S.op("act", lambda e: e.activation(L[2][:], t1[:], AF.Sigmoid), reads=[Rt["t1"]], writes=[r_L[2]])
            for hp in range(4):
                shift_load(hp, s_, r_s, Rt["r_s"])
                shift_load(4 + hp, s_, k_s, Rt["k_s"])
                shift_load(8 + hp, s_, v_s, Rt["v_s"])
                lo, hi = slice(0, 64), slice(64, 128)
                mm_sig(a_f, Rt["a_f"], 1, lo, vc[:, 2, hp:hp + 1], hp, AF.Sigmoid)
                mm_sig(a_b, Rt["a_b"], 1, hi, vc[:, 3, hp:hp + 1], hp, AF.Sigmoid)
                S.op("act", lambda e: e.mul(t1[:], k_s[:], vc[:, 4, hp:hp + 1]), reads=[Rt["k_s"], r_vc], writes=[Rt["t1"]])
                S.op("act", lambda e: e.activation(t2[:], t1[:], AF.Square), reads=[Rt["t1"]], writes=[Rt["t2"]])
                bones_mm(t2, Rt["t2"], lambda c, b: S.op(
                    "act", lambda e: e.activation(kkn[:, c * 512:(c + 1) * 512], PB[b][:], AF.Sqrt, bias=1e-12, scale=1.0),
                    reads=[RP[b]], writes=[Rt["kkn"]], add=(c > 0)))
                S.op("dve", lambda e: e.reciprocal(kkn[:], kkn[:]), reads=[Rt["kkn"]], writes=[Rt["kkn"]])
                S.op("dve", lambda e: e.tensor_tensor(kkn[:], kkn[:], t1[:], ALU.mult), reads=[Rt["kkn"], Rt["t1"]], writes=[Rt["kkn"]])
                S.op("dve", lambda e: e.tensor_scalar(t2[:], a_f[:], vc[:, 5, hp:hp + 1], omka[:, hp:hp + 1], ALU.mult, ALU.add),
                     reads=[Rt["a_f"], r_vc], writes=[Rt["t2"]])
                S.op("dve", lambda e: e.tensor_tensor(t2[:], t2[:], k_s[:], ALU.mult), reads=[Rt["t2"], Rt["k_s"]], writes=[Rt["t2"]])
                S.op("dve", lambda e: e.tensor_scalar(t1[:], a_b[:], vc[:, 5, hp:hp + 1], omka[:, hp:hp + 1], ALU.mult, ALU.add),
                     reads=[Rt["a_b"], r_vc], writes=[Rt["t1"]])
                S.op("dve", lambda e: e.tensor_tensor(k_s[:], t1[:], k_s[:], ALU.mult), reads=[Rt["t1"], Rt["k_s"]], writes=[Rt["k_s"]])
                S.op("dve", lambda e: e.tensor_tensor(t1[:], t2[:], k_s[:], ALU.add), reads=[Rt["t2"], Rt["k_s"]], writes=[Rt["t1"]])
                S.op("dve", lambda e: e.scalar_tensor_tensor(t1[:], t1[:], vc[:, 6, hp:hp + 1], r_s[:], ALU.mult, ALU.mult),
                     reads=[Rt["t1"], r_vc, Rt["r_s"]], writes=[Rt["t1"]])
                bones_mm(t1, Rt["t1"], lambda c, b: S.op(
                    "dve", lambda e: e.tensor_tensor(bonus[:, c * 512:(c + 1) * 512], PB[b][:], v_s[:, c * 512:(c + 1) * 512], ALU.mult),
                    reads=[RP[b], Rt["v_s"]], writes=[Rt["bonus"]], add=(c > 0)))
                for d in range(2):
                    a_d, r_ad = (a_f, Rt["a_f"]) if d == 0 else (a_b, Rt["a_b"])
                    kd, r_kd = (t2, Rt["t2"]) if d == 0 else (k_s, Rt["k_s"])
                    mm_sig(sg, Rt["sg"], 0, lo if d == 0 else hi, vc[:, d, hp:hp + 1], hp, AF.Sigmoid)
                    S.op("dve", lambda e: e.tensor_tensor_scan(t1[:], scm[:], sg[:], 0.0, ALU.mult, ALU.add),
                         reads=[r_scm, Rt["sg"]], writes=[Rt["t1"]])
                    S.op("dve", lambda e: e.tensor_copy(totb[:].unsqueeze(2), v3(t1[:])[:, :, 63:64]), reads=[Rt["t1"]], writes=[r_totb])
                    if d == 1:
                        S.op("dve", lambda e: e.tensor_tensor(t1[:], sg[:], t1[:], ALU.subtract), reads=[Rt["sg"], Rt["t1"]], writes=[Rt["t1"]])
                        S.op("dve", lambda e: e.tensor_tensor(v3(t1[:]), v3(t1[:]), totb[:].unsqueeze(2).broadcast_to([128, 32, 64]), ALU.add),
                             reads=[Rt["t1"], r_totb], writes=[Rt["t1"]])
                    S.op("act", lambda e: e.activation(PCs[d][:], totb[:], AF.Exp, scale=-KAPPA), reads=[r_totb], writes=[r_PCs[d]])
                    S.op("dve", lambda e: e.tensor_tensor(sg[:], t1[:], sg[:], ALU.subtract), reads=[Rt["sg"], Rt["t1"]], writes=[Rt["sg"]])
                    S.op("act", lambda e: e.activation(sg[:], sg[:], AF.Exp, scale=-KAPPA), reads=[Rt["sg"]], writes=[Rt["sg"]])
                    S.op("act", lambda e: e.activation(X, t1[:], AF.Exp, scale=KAPPA), reads=[Rt["t1"]], writes=[r_Pt])
                    S.op("act", lambda e: e.activation(t1[:], t1[:], AF.Exp, scale=-KAPPA), reads=[Rt["t1"]], writes=[Rt["t1"]])
                    S.op("dve", lambda e: e.tensor_tensor(a_d[:], a_d[:], kkn[:], ALU.mult), reads=[r_ad, Rt["kkn"]], writes=[r_ad])
                    S.op("dve", lambda e: e.tensor_tensor(HAT[d][:, :, 0, :], v3(a_d[:]), v3(X), ALU.mult),
                         reads=[r_ad, r_Pt], writes=[r_HAT[d]])
                    S.op("dve", lambda e: e.tensor_tensor(HAT[d][:, :, 1, :], v3(kd[:]), v3(X), ALU.mult),
                         reads=[r_kd, r_Pt], writes=[r_HAT[d]], add=True)
                    S.op("dve", lambda e: e.scalar_tensor_tensor(HAT[d][:, :, 2, :], v3(kkn[:]), -1.0, v3(sg[:]), ALU.mult, ALU.mult),
                         reads=[Rt["kkn"], Rt["sg"]], writes=[r_HAT[d]], add=True)
                    S.op("dve", lambda e: e.tensor_tensor(HAT[d][:, :, 3, :], v3(r_s[:]), v3(t1[:]), ALU.mult),
                         reads=[Rt["r_s"], Rt["t1"]], writes=[r_HAT[d]], add=True)
                S.op("pool", lambda e: e.memset(STs[:], 0.0), writes=[r_STs])
                S.op("pool", lambda e: e.memset(ytm[:], 0.0), writes=[r_ytm])
                S.op("act", lambda e: e.copy(vb[:], v_s[:]), reads=[Rt["v_s"]], writes=[r_vb])
                mm_sig(r_s, Rt["r_s"], 2, slice(0, 128), None, hp, None)
                HS = (slice(0, 64), slice(64, 128))
                UN = ((0, 0), (1, 0), (0, 1), (1, 1))

                def v2(ap, n=128):
                    return ap.rearrange("p (d x) -> p d x", x=n)

                def pre_pe(i):
                    cidx = (i, 31 - i)
                    G["conv_step"](1)
                    first = True
                    for d, hl in UN:
                        hs = HS[hl]
                        ci = cidx[d]
                        S.op("pe", lambda e: e.matmul(PB[0][hs, 256 + d * 128 + hl * 64:256 + d * 128 + hl * 64 + 64], HAT[d][hs, ci, 2, :], identb[hs, hs], start=True, stop=True),
                             reads=[r_HAT[d], r_idb], writes=[RP[0]], add=not first)
                        S.op("pe", lambda e: e.matmul(PB[1][hs, 256 + d * 64:256 + d * 64 + 64], HAT[d][hs, ci, 0, :], identb[hs, hs], start=True, stop=True),
                             reads=[r_HAT[d], r_idb], writes=[RP[1]], add=not first)
                        S.op("pe", lambda e: e.matmul(PB[1][hs, 384 + d * 64:384 + d * 64 + 64], vb[hs, ci * 64:(ci + 1) * 64], identb[hs, hs], start=True, stop=True),
                             reads=[r_vb, r_idb], writes=[RP[1]], add=True)
                        S.op("pe", lambda e: e.matmul(PB[0][hs, d * 128:(d + 1) * 128], HAT[d][hs, ci, 0, :],
                                                      HAT[d][hs, ci, 2:4, :].rearrange("p a t -> p (a t)"), start=True, stop=True),
                             reads=[r_HAT[d]], writes=[RP[0]], add=True)
                        S.op("pe", lambda e: e.matmul(PB[1][hs, d * 128:(d + 1) * 128], HAT[d][hs, ci, 2, :],
                                                      HAT[d][hs, ci, 0:2, :].rearrange("p a t -> p (a t)"), start=True, stop=True),
                             reads=[r_HAT[d]], writes=[RP[1]], add=True)
                        first = False

                def pre_ev(i):
                    p = i % 2
                    cidx = (i, 31 - i)
                    P3 = v2(PB[0][:, 256:512])
                    T0v = v2(T0[:])
                    T1v = v2(T1[:])
                    S.op("act", lambda e: e.copy(T0[:], PB[0][:, 0:256]), reads=[RP[0]], writes=[r_T0])
                    for hl in range(2):
                        hs = HS[hl]
                        cs = slice(hl * 64, (hl + 1) * 64)
                        S.op("act", lambda e: e.copy(ATbd[p][hs, :, cs], P3[hs, :, cs]), reads=[RP[0]], writes=[r_ATbd[p]], add=(hl > 0))
                    S.op("act", lambda e: e.copy(T1[:], PB[1][:, 0:256]), reads=[RP[1]], writes=[r_T1])
                    S.op("act", lambda e: e.copy(Xbs[p][:, :, 64:128], v2(PB[1][:, 256:384], 64)), reads=[RP[1]], writes=[r_Xbs[p]])
                    S.op("act", lambda e: e.copy(Vst[p][:], v2(PB[1][:, 384:512], 64)), reads=[RP[1]], writes=[r_Vst[p]])
                    S.op("pool", lambda e: e.tensor_tensor(Xbs[p][:, :, 0:64], T0v[:, :, 64:128], RM[:, 1], ALU.mult), reads=[r_T0, r_RM], writes=[r_Xbs[p]], add=True)
                    for hl in range(2):
                        hs = HS[hl]
                        cs = slice(hl * 64, (hl + 1) * 64)
                        S.op("pool", lambda e: e.tensor_tensor(Nst[p][hs, :, cs], T1v[hs, :, 0:64], RM[hs, 2], ALU.mult), reads=[r_T1, r_RM], writes=[r_Nst[p]], add=(hl > 0))
                    for hl in range(2):
                        hs = HS[hl]
                        cs = slice(hl * 64, (hl + 1) * 64)
                        S.op("pool", lambda e: e.tensor_tensor(Ast[p][hs, :, cs], T0v[hs, :, 0:64], RM[hs, 0], ALU.mult), reads=[r_T0, r_RM], writes=[r_Ast[p]], add=(hl > 0))
                    for hl in range(2):
                        hs = HS[hl]
                        cs = slice(hl * 64, (hl + 1) * 64)
                        S.op("pool", lambda e: e.tensor_tensor(Akabd[p][hs, :, cs], T1v[hs, :, 64:128], RM[hs, 2], ALU.mult), reads=[r_T1, r_RM], writes=[r_Akabd[p]], add=(hl > 0))
                    first = True
                    for d, hl in UN:
                        hs = HS[hl]
                        S.op("pool", lambda e: e.tensor_copy(KD[p][hs, d, hl * 64:(hl + 1) * 64], HAT[d][hs, cidx[d], 1, :]),
                             reads=[r_HAT[d]], writes=[r_KD[p]], add=not first)
                        first = False

                def seed(i):
                    p = i % 2
                    S.op("pe", lambda e: e.matmul(PB[2][:, 0:256], identb[:], Xbs[p][:].rearrange("p d x -> p (d x)"), start=True, stop=False),
                         reads=[r_idb, r_Xbs[p]], writes=[RP[2]])

                def level_pe(i, k):
                    p = i % 2
                    q = k % 2
                    if k == 0:
                        Nq, r_Nq, Aq, r_Aq = Nst[p], r_Nst[p], Ast[p], r_Ast[p]
                    else:
                        Nq, r_Nq, Aq, r_Aq = Nbd[q], r_Nbd[q], Abd[q], r_Abd[q]
                    if k < 5:
                        for d in range(2):
                            S.op("pe", lambda e: e.matmul(PB[3][:, d * 128:(d + 1) * 128], Aq[:, d, :], Nq[:, d, :], start=True, stop=True),
                                 reads=[r_Nq, r_Aq], writes=[RP[3]], add=(d > 0))
                    for d in range(2):
                        S.op("pe", lambda e: e.matmul(PB[2][:, d * 128:(d + 1) * 128], Nq[:, d, :], Xbs[p][:, d, :], start=False, stop=(k == 5)),
                             reads=[r_Nq, r_Xbs[p]], writes=[RP[2]], add=True)
                    if k < 4:
                        for d in range(2):
                            S.op("pe", lambda e: e.matmul(PB[4][:, d * 128:(d + 1) * 128], Nq[:, d, :], Aq[:, d, :], start=True, stop=True),
                                 reads=[r_Nq, r_Aq], writes=[RP[4]], add=(d > 0))

                def level_ev(i, k):
                    p = i % 2
                    q = (k + 1) % 2
                    if k < 5:
                        S.op("dve", lambda e: e.tensor_copy(Nbd[q][:].rearrange("p d x -> p (d x)"), PB[3][:, 0:256]), reads=[RP[3]], writes=[r_Nbd[q]])
                    S.op("act", lambda e: e.copy(Xbs[p][:].rearrange("p d x -> p (d x)"), PB[2][:, 0:256]), reads=[RP[2]], writes=[r_Xbs[p]])
                    if k < 4:
                        S.op("act", lambda e: e.copy(Abd[q][:].rearrange("p d x -> p (d x)"), PB[4][:, 0:256]), reads=[RP[4]], writes=[r_Abd[q]])

                def post1_pe(i):
                    p = i % 2
                    cidx = (i, 31 - i)
                    for d in range(2):
                        ci = cidx[d]
                        o5 = PB[5][:, d * 128:(d + 1) * 128]
                        o6 = PB[6][:, d * 128:(d + 1) * 128]
                        S.op("pe", lambda e: e.matmul(o5, ATbd[p][:, d, :], Xbs[p][:, d, :], start=True, stop=False),
                             reads=[r_ATbd[p], r_Xbs[p]], writes=[RP[5]], add=(d > 0))
                        S.op("pe", lambda e: e.matmul(o5[:, 0:64], identb[:], HAT[d][:, ci, 3, :], start=False, stop=False),
                             reads=[r_idb, r_HAT[d]], writes=[RP[5]], add=True)
                        S.op("pe", lambda e: e.matmul(o5[:, 64:128], identb[:], I2b[:], start=False, stop=True),
                             reads=[r_idb, r_cst], writes=[RP[5]], add=True)
                        S.op("pe", lambda e: e.matmul(o6, Akabd[p][:, d, :], Xbs[p][:, d, :], start=True, stop=False),
                             reads=[r_Akabd[p], r_Xbs[p]], writes=[RP[6]], add=(d > 0))
                        S.op("pe", lambda e: e.matmul(o6[:, 0:64], KD[p][:, d, :], HAT[d][:, ci, 3, :], start=False, stop=False),
                             reads=[r_KD[p], r_HAT[d]], writes=[RP[6]], add=True)
                        S.op("pe", lambda e: e.matmul(o6[:, 64:128], KD[p][:, d, :], I2b[:], start=False, stop=True),
                             reads=[r_KD[p], r_cst], writes=[RP[6]], add=True)

                def post1_ev(i):
                    P5 = PB[5][:, 0:256].rearrange("p (d a x) -> p d a x", a=2, x=64)
                    P6 = PB[6][:, 0:256].rearrange("p (d a x) -> p d a x", a=2, x=64)
                    for hl in range(2):
                        hs = HS[hl]
                        cs = slice(hl * 64, (hl + 1) * 64)
                        S.op("act", lambda e: e.copy(QM1bd[hs, :, :, cs], P5[hs]), reads=[RP[5]], writes=[r_QM1bd], add=(hl > 0))
                    for hl in range(2):
                        hs = HS[hl]
                        cs = slice(hl * 64, (hl + 1) * 64)
                        S.op("dve", lambda e: e.tensor_tensor(QM2bd[hs, :, :, cs], P6[hs], MQ2[hs], ALU.mult), reads=[RP[6], r_cst], writes=[r_QM2bd], add=(hl > 0))

                def scan_pe(i):
                    p = i % 2
                    for d in range(2):
                        for a_ in range(2):
                            o_ = PB[7][:, a_ * 128 + d * 64:a_ * 128 + (d + 1) * 64]
                            S.op("pe", lambda e: e.matmul(o_, QM1bd[:, d, a_, :], STs[:, d, :], start=True, stop=False),
                                 reads=[r_QM1bd, r_STs], writes=[RP[7]], add=not (d == 0 and a_ == 0))
                            S.op("pe", lambda e: e.matmul(o_, QM2bd[:, d, a_, :], Vst[p][:, d, :], start=False, stop=True),
                                 reads=[r_QM2bd, r_Vst[p]], writes=[RP[7]], add=True)

                def scan_ev(i):
                    cidx = (i, 31 - i)
                    S.op("dve", lambda e: e.tensor_copy(ytmp[:], PB[7][:, 0:128]), reads=[RP[7]], writes=[r_ytmp])
                    for d in range(2):
                        ci = cidx[d]
                        S.op("act", lambda e: e.mul(STs[:, d, :], PB[7][:, 128 + d * 64:128 + (d + 1) * 64], PCs[d][:, ci:ci + 1]),
                             reads=[RP[7], r_PCs[d]], writes=[r_STs], add=(d > 0))
                    for d in range(2):
                        ci = cidx[d]
                        S.op("pool", lambda e: e.tensor_tensor(ytm[:, ci, :], ytmp[:, d * 64:(d + 1) * 64], ytm[:, ci, :], ALU.add),
                             reads=[r_ytmp, r_ytm], writes=[r_ytm])

                pre_pe(0)
                pre_ev(0)
                seed(0)
                pre_pe(1)
                pre_ev(1)
                for k in range(6):
                    level_pe(0, k)
                    level_ev(0, k)
                seed(1)
                for i in range(32):
                    c = i + 1
                    has_c = c < 32
                    has_n = c + 1 < 32
                    if has_c:
                        level_pe(c, 0)
                    post1_pe(i)
                    if has_c:
                        level_ev(c, 0)
                    post1_ev(i)
                    if has_c:
                        level_pe(c, 1)
                    scan_pe(i)
                    if has_c:
                        level_ev(c, 1)
                    scan_ev(i)
                    if has_c:
                        level_pe(c, 2)
                    if has_n:
                        pre_pe(c + 1)
                    if has_c:
                        level_ev(c, 2)
                    if has_n:
                        pre_ev(c + 1)
                    if has_c:
                        for k in range(3, 6):
                            level_pe(c, k)
                            level_ev(c, k)
                    if has_n:
                        seed(c + 1)
                sq = t2[:].rearrange("p (g f) -> p g f", f=64)
                bc = lambda ap: ap.unsqueeze(2).broadcast_to([128, 32, 64])
                Vg = lambda f: S.op("dve", f, reads=[r_ytm, r_gs, Rt["t2"]], writes=[r_ytm, r_gs])
                Vg(lambda e: e.tensor_reduce(gs[:, 0, :], ytm[:], AX.X, ALU.add))
                Vg(lambda e: e.tensor_scalar(gs[:, 0, :], gs[:, 0, :], 1.0 / 64, None, ALU.mult))
                Vg(lambda e: e.tensor_tensor(ytm[:], ytm[:], bc(gs[:, 0, :]), ALU.subtract))
                S.op("act", lambda e: e.activation(sq, ytm[:], AF.Square), reads=[r_ytm], writes=[Rt["t2"]])
                Vg(lambda e: e.tensor_reduce(gs[:, 1, :], sq, AX.X, ALU.add))
                S.op("act", lambda e: e.activation(gs[:, 2, :], gs[:, 1, :], AF.Sqrt, bias=GN_EPS, scale=1.0 / 64), reads=[r_gs], writes=[r_gs])
                Vg(lambda e: e.reciprocal(gs[:, 2, :], gs[:, 2, :]))
                Vg(lambda e: e.tensor_tensor(ytm[:], ytm[:], bc(gs[:, 2, :]), ALU.mult))
                for c0_ in range(0, 32, 8):
                    b = (c0_ // 8) % 2
                    first = True
                    for hl in range(2):
                        hs = HS[hl]
                        for q in range(8):
                            S.op("pe", lambda e: e.matmul(PB[b][hs, q * 64:(q + 1) * 64], ytm[hs, c0_ + q, :], identf[hs, hs], start=True, stop=True),
                                 reads=[r_ytm, r_idf], writes=[RP[b]], add=not first)
                            first = False
                    S.op("act", lambda e: e.activation(t1[:, c0_ * 64:(c0_ + 8) * 64], PB[b][:], AF.Identity,
                                                       bias=vc[:, 8, hp:hp + 1], scale=vc[:, 7, hp:hp + 1]),
                         reads=[RP[b], r_vc], writes=[Rt["t1"]], add=(c0_ > 0))
                S.op("dve", lambda e: e.tensor_tensor(t1[:], t1[:], bonus[:], ALU.add), reads=[Rt["t1"], Rt["bonus"]], writes=[Rt["t1"]])
                S.op("dve", lambda e: e.tensor_tensor(yo[:], t1[:], r_s[:], ALU.mult), reads=[Rt["t1"], Rt["r_s"]], writes=[r_yo])
                S.dma("pool", ya_d[hp * 128:(hp + 1) * 128, s_ * T:(s_ + 1) * T], yo[:], reads=[r_yo])
        S.barrier()


def PHASE_D(nc, S, PB, RP, G):
    x, w_in, w_oa, w_ob, w_out, bg = G["x"], G["w_in"], G["w_oa"], G["w_ob"], G["w_out"], G["bg"]
    hT_d, ya_d, yb_d, xm_d, evac = G["hT_d"], G["ya_d"], G["yb_d"], G["xm_d"], G["evac"]
    with ExitStack() as st:
        def sb(n, s, d):
            return st.enter_context(nc.sbuf_tensor(n, s, d))
        woa = sb("woa", [128, 4, D], BF16); r_woa = Res()
        wob = sb("wob", [128, 4, D], BF16); r_wob = Res()
        wg = sb("wg", [128, 8, 2048], BF16); r_wg = Res()
        wo = sb("wo", [128, 8, D], BF16); r_wo = Res()
        bgt = sb("bgt", [128, 16], F32); r_bg = Res()
        S.dma("sp", bgt[:], bg, writes=[r_bg])
        S.dma("pool", woa[:], w_oa.rearrange("(k p) n -> p k n", p=128), writes=[r_woa])
        S.dma("pool", wob[:], w_ob.rearrange("(k p) n -> p k n", p=128), writes=[r_wob])
        for k in range(8):
            S.dma("pool", wg[:, k, :], w_in[k * 128:(k + 1) * 128, 3456:5504], writes=[r_wg], add=True)
            S.dma("pool", wo[:, k, :], w_out[k * 128:(k + 1) * 128, :], writes=[r_wo], add=True)
        ra = sb("ra", [128, 4, 512], BF16); r_ra = Res()
        rb = sb("rb", [128, 4, 512], BF16); r_rb = Res()
        hTb = sb("hTb", [128, 8, 512], BF16); r_hTb = Res()
        xt = sb("xtD", [128, 4, D], F32); r_xt = Res()
        xm = sb("xmD", [128, 4, D], F32); r_xm = Res()
        zT = sb("zT", [128, 8, 512], BF16); r_zT = [Res() for _ in range(8)]
        sga = [sb(f"sga{i}", [128, 512], F32) for i in range(2)]; r_sga = [Res(), Res()]
        sgb = [sb(f"sgb{i}", [128, 512], F32) for i in range(2)]; r_sgb = [Res(), Res()]
        z1 = [sb(f"z1{i}", [128, 512], F32) for i in range(2)]; r_z1 = [Res(), Res()]
        z2 = [sb(f"z2{i}", [128, 512], F32) for i in range(2)]; r_z2 = [Res(), Res()]
        for tb in range(8):
            tsl = slice(tb * 512, (tb + 1) * 512)
            S.dma("sp", ra[:], ya_d.rearrange("(k p) t -> p k t", p=128)[:, :, tsl], writes=[r_ra])
            S.dma("sp", rb[:], yb_d.rearrange("(k p) t -> p k t", p=128)[:, :, tsl], writes=[r_rb])
            S.dma("sp", hTb[:], hT_d.rearrange("(k p) t -> p k t", p=128)[:, :, tsl], writes=[r_hTb])
            S.dma("sp", xt[:], x[tsl, :].rearrange("(j p) d -> p j d", p=128), writes=[r_xt])
            for fc in range(8):
                i = fc % 2
                b0 = 4 * i
                fsl = slice(fc * 128, (fc + 1) * 128)
                for k in range(4):
                    S.op("pe", lambda e: e.matmul(PB[b0][:], woa[:, k, fsl], ra[:, k, :], start=(k == 0), stop=(k == 3)),
                         reads=[r_woa, r_ra], writes=[RP[b0]], add=(k > 0))
                for k in range(4):
                    S.op("pe", lambda e: e.matmul(PB[b0 + 1][:], wob[:, k, fsl], rb[:, k, :], start=(k == 0), stop=(k == 3)),
                         reads=[r_wob, r_rb], writes=[RP[b0 + 1]], add=(k > 0))
                for k in range(8):
                    S.op("pe", lambda e: e.matmul(PB[b0 + 2][:], wg[:, k, fsl], hTb[:, k, :], start=(k == 0), stop=(k == 7)),
                         reads=[r_wg, r_hTb], writes=[RP[b0 + 2]], add=(k > 0))
                for k in range(8):
                    S.op("pe", lambda e: e.matmul(PB[b0 + 3][:], wg[:, k, 1024 + fc * 128:1024 + (fc + 1) * 128], hTb[:, k, :],
                                                  start=(k == 0), stop=(k == 7)),
                         reads=[r_wg, r_hTb], writes=[RP[b0 + 3]], add=(k > 0))
                S.op("act", lambda e: e.activation(sga[i][:], PB[b0 + 2][:], AF.Sigmoid, bias=bgt[:, fc:fc + 1]),
                     reads=[RP[b0 + 2], r_bg], writes=[r_sga[i]])
                S.op("act", lambda e: e.activation(sgb[i][:], PB[b0 + 3][:], AF.Sigmoid, bias=bgt[:, 8 + fc:9 + fc]),
                     reads=[RP[b0 + 3], r_bg], writes=[r_sgb[i]])
                S.op("dve", lambda e: e.tensor_tensor(z1[i][:], PB[b0][:], sga[i][:], ALU.mult),
                     reads=[RP[b0], r_sga[i]], writes=[r_z1[i]])
                S.op("dve", lambda e: e.tensor_tensor(z2[i][:], PB[b0 + 1][:], sgb[i][:], ALU.mult),
                     reads=[RP[b0 + 1], r_sgb[i]], writes=[r_z2[i]])
                S.op("pool", lambda e: e.tensor_tensor(zT[:, fc, :], z1[i][:], z2[i][:], ALU.add),
                     reads=[r_z1[i], r_z2[i]], writes=[r_zT[fc]])
            n = 0
            for j in range(4):
                for half in range(2):
                    b = n % 8
                    n += 1
                    for k in range(8):
                        S.op("pe", lambda e: e.matmul(PB[b][:], zT[:, k, j * 128:(j + 1) * 128],
                                                      wo[:, k, half * 512:(half + 1) * 512], start=(k == 0), stop=(k == 7)),
                             reads=[r_zT[k], r_wo], writes=[RP[b]], add=(k > 0))
                    S.op("dve", lambda e: e.tensor_tensor(xm[:, j, half * 512:(half + 1) * 512], PB[b][:],
                                                          xt[:, j, half * 512:(half + 1) * 512], ALU.add),
                         reads=[RP[b], r_xt], writes=[r_xm], add=(n > 1))
            S.dma("pool", xm_d[tsl, :].rearrange("(j p) d -> p j d", p=128), xm[:], reads=[r_xm])
        S.barrier()


def PHASE_E(nc, S, PB, RP, G):
    g2, wr, br, wgu_s, wdn_s = G["g2"], G["wr"], G["br"], G["wgu_s"], G["wdn_s"]
    xm_d, out, evac = G["xm_d"], G["out"], G["evac"]
    identf, r_idf = G["identf"], G["r_idf"]
    identb, r_idb = G["identb"], G["r_idb"]
    S.barrier()
    with ExitStack() as st:
        def sb(n, s, d):
            return st.enter_context(nc.sbuf_tensor(n, s, d))
        g2t = sb("g2t", [128, D], F32); r_g2 = Res()
        S.dma("sp", g2t[:], g2.broadcast_to([128, D]), writes=[r_g2])
        wrt = sb("wrt", [128, 8, 36], F32); r_wr = Res()
        S.dma("sp", wrt[:], wr.rearrange("(k p) n -> p k n", p=128), writes=[r_wr])
        brt = sb("brt", [128, 36], F32); r_br = Res()
        S.dma("sp", brt[:], br.broadcast_to([128, 36]), writes=[r_br])
        xm = sb("xmE", [128, 4, D], F32); r_xm = Res()
        junk = sb("junkE", [128, D], F32)
        ss = sb("ssE", [128, 4], F32); r_ss = Res()
        h2 = sb("h2", [128, D], F32); r_h2 = Res()
        h2T32 = sb("h2T32", [128, 8, 128], F32); r_h2T32 = Res()
        h2Tbs = [sb(f"h2Tb{i}", [128, 8, 512], BF16) for i in range(2)]; r_h2Tbs = [Res(), Res()]
        lg = sb("lg", [128, 36], F32); r_lg = Res()
        sm = sb("sm", [128, 64], F32); r_sm = Res()
        comb = sb("comb", [128, 128], F32); r_comb = Res()
        S.op("pool", lambda e: e.memset(comb[:], 0.0), writes=[r_comb])
        cTs = [sb(f"cT{i}", [128, 512], F32) for i in range(2)]; r_cTs = [Res(), Res()]
        cHs = [sb(f"cH{i}", [128, 512], BF16) for i in range(2)]; cLs = [sb(f"cL{i}", [128, 512], BF16) for i in range(2)]
        cD = sb("cD", [128, 128], F32); r_cD = Res()
        bc = [sb(f"bc{i}", [128, 512], F32) for i in range(2)]; r_bc = [Res(), Res()]
        NWG = 6
        wgu = [sb(f"wgu{i}", [128, 8, 512], BF16) for i in range(NWG)]; r_wgu = [Res() for _ in range(NWG)]
        wdn = [sb(f"wdn{i}", [128, 8, D], BF16) for i in range(2)]; r_wdn = [Res() for _ in range(2)]
        heT = [sb(f"heT{i}", [128, 8, 512], BF16) for i in range(2)]; r_heT = [Res(), Res()]
        sg = [sb(f"sgE{i}", [128, 512], F32) for i in range(2)]; r_sg = [Res(), Res()]
        tt = [sb(f"ttE{i}", [128, 512], F32) for i in range(2)]; r_tt = [Res(), Res()]
        yaccs = [sb(f"yacc{i}", [128, 4, D], F32) for i in range(2)]; r_yas = [Res(), Res()]
        st_ = {"nwg": 0}

        def router_gen(tb):
            h2Tb, r_h2Tb = h2Tbs[tb % 2], r_h2Tbs[tb % 2]
            cT, r_cT = cTs[tb % 2], r_cTs[tb % 2]
            cH, cL = cHs[tb % 2], cLs[tb % 2]
            tsl = slice(tb * 512, (tb + 1) * 512)
            S.dma("pool", xm[:], xm_d[tsl, :].rearrange("(j p) d -> p j d", p=128), writes=[r_xm]); yield
            for _ in range(20):
                yield
            for j in range(4):
                S.op("act", lambda e: e.activation(junk[:], xm[:, j, :], AF.Square, accum_out=ss[:, j:j + 1]),
                     reads=[r_xm], writes=[r_ss], add=(j > 0)); yield
            S.op("act", lambda e: e.activation(ss[:], ss[:], AF.Sqrt, bias=1e-6, scale=1.0 / D), reads=[r_ss], writes=[r_ss]); yield
            S.op("dve", lambda e: e.reciprocal(ss[:], ss[:]), reads=[r_ss], writes=[r_ss]); yield
            for _ in range(4):
                yield
            for j in range(4):
                S.op("dve", lambda e: e.scalar_tensor_tensor(h2[:], xm[:, j, :], ss[:, j:j + 1], g2t[:], ALU.mult, ALU.mult),
                     reads=[r_xm, r_ss, r_g2], writes=[r_h2]); yield
                yield
                yield
                for k0 in range(0, 8, 4):
                    for k in range(k0, k0 + 4):
                        S.op("pe", lambda e: e.matmul(PB[7][:, (k - k0) * 128:(k - k0 + 1) * 128], h2[:, k * 128:(k + 1) * 128], identf[:],
                                                      start=True, stop=True),
                             reads=[r_h2, r_idf], writes=[RP[7]], add=(k > k0))
                    yield
                    S.op("act", lambda e: e.copy(h2T32[:, k0:k0 + 4, :], PB[7][:].rearrange("p (k t) -> p k t", t=128)),
                         reads=[RP[7]], writes=[r_h2T32], add=(k0 > 0)); yield
                    S.op("dve", lambda e: e.tensor_copy(h2Tb[:, k0:k0 + 4, j * 128:(j + 1) * 128], PB[7][:].rearrange("p (k t) -> p k t", t=128)),
                         reads=[RP[7]], writes=[r_h2Tb], add=not (j == 0 and k0 == 0)); yield
                yield
                yield
                yield
                for k in range(8):
                    S.op("pe", lambda e: e.matmul(PB[7][:, 0:36], h2T32[:, k, :], wrt[:, k, :], start=(k == 0), stop=(k == 7)),
                         reads=[r_h2T32, r_wr], writes=[RP[7]], add=(k > 0))
                yield
                ops = []
                g = 0
                V = lambda f: ops.append(f)
                V(lambda e, g=g: e.tensor_tensor(lg[:], PB[7][:, 0:36], brt[:], ALU.add))
                V(lambda e, g=g: e.tensor_reduce(sm[:, 0:1], lg[:, 0:4], AX.X, ALU.max))
                V(lambda e, g=g: e.tensor_scalar(sm[:, 4:8], lg[:, 0:4], sm[:, 0:1], None, ALU.is_equal))
                V(lambda e, g=g: e.tensor_scalar(sm[:, 8:12], lg[:, 0:4], sm[:, 0:1], None, ALU.subtract))
                ops.append(("act", lambda e: e.activation(sm[:, 8:12], sm[:, 8:12], AF.Exp, accum_out=sm[:, 1:2])))
                V(lambda e, g=g: e.reciprocal(sm[:, 2:3], sm[:, 1:2]))
                V(lambda e, g=g: e.tensor_scalar(sm[:, 16:24], lg[:, 4:12], sm[:, 4:5], None, ALU.mult))
                for g in range(1, 4):
                    V(lambda e, g=g: e.scalar_tensor_tensor(sm[:, 16:24], lg[:, 4 + 8 * g:12 + 8 * g], sm[:, 4 + g:5 + g],
                                                       sm[:, 16:24], ALU.mult, ALU.add))
                V(lambda e, g=g: e.tensor_reduce(sm[:, 3:4], sm[:, 16:24], AX.X, ALU.max))
                V(lambda e, g=g: e.tensor_scalar(sm[:, 24:32], sm[:, 16:24], sm[:, 3:4], None, ALU.is_equal))
                V(lambda e, g=g: e.scalar_tensor_tensor(sm[:, 32:40], sm[:, 24:32], -1e30, sm[:, 16:24], ALU.mult, ALU.add))
                V(lambda e, g=g: e.tensor_reduce(sm[:, 12:13], sm[:, 32:40], AX.X, ALU.max))
                V(lambda e, g=g: e.tensor_scalar(sm[:, 40:48], sm[:, 32:40], sm[:, 12:13], None, ALU.is_equal))
                V(lambda e, g=g: e.tensor_tensor(sm[:, 13:14], sm[:, 12:13], sm[:, 3:4], ALU.subtract))
                ops.append(("act", lambda e: e.activation(sm[:, 13:14], sm[:, 13:14], AF.Exp)))
                V(lambda e, g=g: e.tensor_scalar(sm[:, 14:15], sm[:, 13:14], 1.0, None, ALU.add))
                V(lambda e, g=g: e.reciprocal(sm[:, 14:15], sm[:, 14:15]))
                V(lambda e, g=g: e.tensor_tensor(sm[:, 15:16], sm[:, 14:15], sm[:, 13:14], ALU.mult))
                V(lambda e, g=g: e.tensor_tensor(sm[:, 14:15], sm[:, 14:15], sm[:, 2:3], ALU.mult))
                V(lambda e, g=g: e.tensor_tensor(sm[:, 15:16], sm[:, 15:16], sm[:, 2:3], ALU.mult))
                V(lambda e, g=g: e.tensor_scalar(sm[:, 48:56], sm[:, 24:32], sm[:, 14:15], None, ALU.mult))
                V(lambda e, g=g: e.scalar_tensor_tensor(sm[:, 48:56], sm[:, 40:48], sm[:, 15:16], sm[:, 48:56], ALU.mult, ALU.add))
                for g in range(4):
                    V(lambda e, g=g: e.tensor_scalar(comb[:, 8 * g:8 * g + 8], sm[:, 48:56], sm[:, 4 + g:5 + g], None, ALU.mult))
                for f in ops:
                    if isinstance(f, tuple):
                        S.op("act", f[1], reads=[r_sm], writes=[r_sm])
                    else:
                        S.op("dve", f, reads=[r_lg, r_sm, r_comb, r_br, RP[7]], writes=[r_lg, r_sm, r_comb])
                    yield
                yield
                yield
                yield
                S.op("pe", lambda e: e.matmul(PB[7][:, 0:128], comb[:], identf[:], start=True, stop=True),
                     reads=[r_comb, r_idf], writes=[RP[7]]); yield
                S.op("act", lambda e: e.copy(cT[:, j * 128:(j + 1) * 128], PB[7][:, 0:128]), reads=[RP[7]], writes=[r_cT],
                     add=(j > 0)); yield
                jsl = slice(j * 128, (j + 1) * 128)
                S.op("dve", lambda e: e.tensor_copy(cH[:, jsl], cT[:, jsl]), reads=[r_cT], writes=[r_cT], add=True); yield
                S.op("dve", lambda e: e.tensor_tensor(cD[:], cT[:, jsl], cH[:, jsl], ALU.subtract), reads=[r_cT], writes=[r_cD]); yield
                S.op("dve", lambda e: e.tensor_copy(cL[:, jsl], cD[:]), reads=[r_cD], writes=[r_cT], add=True); yield

        def expert_group(tb, eg, gen=None):
            h2Tb, r_h2Tb = h2Tbs[tb % 2], r_h2Tbs[tb % 2]
            cT, r_cT = cTs[tb % 2], r_cTs[tb % 2]
            cH, cL = cHs[tb % 2], cLs[tb % 2]
            hi = eg % 2
            dn = wdn[eg % 2]
            S.dma("sp", dn[:].rearrange("p (e c) n -> p e c n", c=2),
                  wdn_s[eg * 4:(eg + 1) * 4].rearrange("e p c n -> p e c n"), writes=[r_wdn[eg % 2]])
            for el in range(4):
                ex = eg * 4 + el
                wi = st_["nwg"] % NWG
                st_["nwg"] += 1
                S.dma("sp", wgu[wi][:], wgu_s[ex], writes=[r_wgu[wi]])
                bi = ex % 2
                S.op("pe", lambda e: e.matmul(PB[6][:], identb[:, ex:ex + 1].broadcast_to([128, 128]), cH[:], start=True, stop=False),
                     reads=[r_idb, r_cT], writes=[RP[6]])
                S.op("pe", lambda e: e.matmul(PB[6][:], identb[:, ex:ex + 1].broadcast_to([128, 128]), cL[:], start=False, stop=True),
                     reads=[r_idb, r_cT], writes=[RP[6]], add=True)
                S.op("act", lambda e: e.copy(bc[bi][:], PB[6][:]), reads=[RP[6]], writes=[r_bc[bi]])
                for c in range(2):
                    pg, pu = (0, 1) if (c == 0) else (2, 3)
                    for k in range(8):
                        S.op("pe", lambda e: e.matmul(PB[pg][:], wgu[wi][:, k, c * 128:(c + 1) * 128], h2Tb[:, k, :],
                                                      start=(k == 0), stop=(k == 7)),
                             reads=[r_wgu[wi], r_h2Tb], writes=[RP[pg]], add=(k > 0))
                    for k in range(8):
                        S.op("pe", lambda e: e.matmul(PB[pu][:], wgu[wi][:, k, 256 + c * 128:256 + (c + 1) * 128],
                                                      h2Tb[:, k, :], start=(k == 0), stop=(k == 7)),
                             reads=[r_wgu[wi], r_h2Tb], writes=[RP[pu]], add=(k > 0))
                    S.op("act", lambda e: e.activation(sg[c][:], PB[pg][:], AF.Silu), reads=[RP[pg]], writes=[r_sg[c]])
                    S.op("dve", lambda e: e.tensor_tensor(tt[c][:], PB[pu][:], sg[c][:], ALU.mult),
                         reads=[RP[pu], r_sg[c]], writes=[r_tt[c]])
                    S.op("pool", lambda e: e.tensor_tensor(heT[hi][:, el * 2 + c, :], tt[c][:], bc[bi][:], ALU.mult),
                         reads=[r_tt[c], r_bc[bi]], writes=[r_heT[hi]], add=(el * 2 + c > 0))
                    if gen is not None:
                        for _ in range(10):
                            next(gen, None)
            n = 0
            for j in range(4):
                for half in range(2):
                    b = 4 + n % 2
                    n += 1
                    for kk in range(8):
                        S.op("pe", lambda e: e.matmul(PB[b][:], heT[hi][:, kk, j * 128:(j + 1) * 128],
                                                      dn[:, kk, half * 512:(half + 1) * 512],
                                                      start=(kk == 0), stop=(kk == 7)),
                             reads=[r_heT[hi], r_wdn[eg % 2]], writes=[RP[b]], add=(kk > 0))
                    hsl = slice(half * 512, (half + 1) * 512)
                    yn, r_yn = yaccs[eg % 2], r_yas[eg % 2]
                    yo_, r_yo_ = yaccs[(eg + 1) % 2], r_yas[(eg + 1) % 2]
                    if eg == 0:
                        S.op("dve", lambda e: e.tensor_tensor(yn[:, j, hsl], PB[b][:], xm[:, j, hsl], ALU.add),
                             reads=[RP[b], r_xm], writes=[r_yn], add=(n > 1))
                    else:
                        S.op("dve", lambda e: e.tensor_tensor(yn[:, j, hsl], PB[b][:], yo_[:, j, hsl], ALU.add),
                             reads=[RP[b], r_yo_], writes=[r_yn], add=(n > 1))

        for _ in router_gen(0):
            pass
        for tb in range(8):
            tsl = slice(tb * 512, (tb + 1) * 512)
            gen = None
            for eg in range(8):
                expert_group(tb, eg, gen)
                if eg == 0 and tb + 1 < 8:
                    gen = router_gen(tb + 1)
            if gen is not None:
                for _ in gen:
                    pass
            S.dma("pool", out[tsl, :].rearrange("(j p) d -> p j d", p=128), yaccs[1][:], reads=[r_yas[1]])
        S.barrier()


def PHASE_C(nc, S, PB, RP, G):
    p_d, yb_d, qkg, btab, nmask, bonesd, evac = G["p_d"], G["yb_d"], G["qkg"], G["btab"], G["nmask"], G["bonesd"], G["evac"]
    identb, r_idb = G["identb"], G["r_idb"]
    with ExitStack() as st:
        def sb(n, s, d):
            return st.enter_context(nc.sbuf_tensor(n, s, d))
        BT = sb("BT", [128, 8, 14, 64], F32); r_BT = Res()
        mk = sb("mk", [128, 64], F32); r_mk = Res()
        gk = sb("gk", [128, 2], F32); r_gk = Res()
        bones = sb("bonesC", [128, 128], F32); r_bones = Res()
        S.dma("sp", BT[:], btab, writes=[r_BT])
        S.dma("sp", mk[:], nmask, writes=[r_mk])
        S.dma("sp", gk[:], qkg, writes=[r_gk])
        S.dma("sp", bones[:], bonesd, writes=[r_bones])
        for h in range(8):
            for pp in range(14):
                S.op("pool", lambda e: e.tensor_tensor(BT[:, h, pp, :], BT[:, h, pp, :], mk[:], ALU.add),
                     reads=[r_BT, r_mk], writes=[r_BT])
        qf = sb("qf", [128, T], F32); r_qf = Res()
        kf = sb("kf", [128, T], F32); r_kf = Res()
        vf = sb("vf", [128, T], F32); r_vf = Res()
        sq = sb("sqC", [128, T], F32); r_sq = Res()
        rq = sb("rqC", [128, T], F32); r_rq = Res()
        qn = sb("qn", [128, T], BF16); r_qn = Res()
        kn = sb("kn", [128, T], BF16); r_kn = Res()
        vb = sb("vb", [128, T], BF16); r_vb = Res()
        Vtm = [sb(f"Vtm{a}", [128, 16, 2, 65], BF16) for a in range(2)]; r_V = [Res(), Res()]
        for a in range(2):
            S.op("pool", lambda e: e.memset(Vtm[a][:], 1.0), writes=[r_V[a]])
        scs = [sb(f"scs{i}", [128, 4, 64], F32) for i in range(3)]; r_scs = [Res() for _ in range(3)]
        Eb = [sb(f"Eb{i}", [128, 4, 64], BF16) for i in range(3)]; r_E = [Res() for _ in range(3)]
        rden = [sb(f"rden{i}", [64, 1], F32) for i in range(2)]; r_rden = [Res(), Res()]
        otm = sb("otm", [64, 32, 128], BF16); r_otm = Res()
        ybT = sb("ybT", [128, T], BF16); r_ybT = Res()
        for s_ in range(2):
            t0 = s_ * T
            for hp in range(4):
                S.dma("sp", qf[:], p_d[1920 + hp * 128:1920 + (hp + 1) * 128, t0:t0 + T], writes=[r_qf])
                S.dma("sp", kf[:], p_d[2432 + hp * 128:2432 + (hp + 1) * 128, t0:t0 + T], writes=[r_kf])
                S.dma("sp", vf[:], p_d[2944 + hp * 128:2944 + (hp + 1) * 128, t0:t0 + T], writes=[r_vf])
                for (src, r_src, dst, r_dst, gi, sc, bi) in ((qf, r_qf, qn, r_qn, 0, 1.0, 64e-6), (kf, r_kf, kn, r_kn, 1, 1.0 / 64, 1e-6)):
                    S.op("act", lambda e: e.activation(sq[:], src[:], AF.Square), reads=[r_src], writes=[r_sq])
                    for c in range(4):
                        b = c % 2
                        S.op("pe", lambda e: e.matmul(PB[b][:], bones[:], sq[:, c * 512:(c + 1) * 512], start=True, stop=True),
                             reads=[r_bones, r_sq], writes=[RP[b]])
                        S.op("act", lambda e: e.activation(rq[:, c * 512:(c + 1) * 512], PB[b][:], AF.Sqrt, bias=bi, scale=sc),
                             reads=[RP[b]], writes=[r_rq], add=(c > 0))
                    S.op("dve", lambda e: e.reciprocal(rq[:], rq[:]), reads=[r_rq], writes=[r_rq])
                    S.op("dve", lambda e: e.scalar_tensor_tensor(dst[:], src[:], gk[:, gi:gi + 1], rq[:], ALU.mult, ALU.mult),
                         reads=[r_src, r_gk, r_rq], writes=[r_dst])
                S.op("act", lambda e: e.copy(vb[:], vf[:]), reads=[r_vf], writes=[r_vb])
                for a in range(2):
                    nb = 16 - a
                    for b0 in range(0, nb, 4):
                        n4 = min(4, nb - b0)
                        pb = 2 + (b0 // 4) % 2
                        for q in range(n4):
                            tk = a * 64 + (b0 + q) * 128
                            S.op("pe", lambda e: e.matmul(PB[pb][:, q * 128:(q + 1) * 128], vb[:, tk:tk + 128], identb[:],
                                                          start=True, stop=True),
                                 reads=[r_vb, r_idb], writes=[RP[pb]], add=(q > 0))
                        evac(Vtm[a][:, b0:b0 + n4, :, 0:64],
                             PB[pb][:, 0:n4 * 128].rearrange("p (b h d) -> p b h d", h=2, d=64),
                             [RP[pb]], [r_V[a]], add=(b0 > 0))
                units = [(hl, i) for hl in range(2) for i in range(32)]

                def na_stage1(n):
                    hl, i = units[n]
                    h = hp * 2 + hl
                    hs = slice(hl * 64, (hl + 1) * 64)
                    start = min(max(i - 4, 0), 24)
                    pp0 = start - i + 7
                    u = n % 3
                    ps = (0, 4, 5)[u]
                    for c in range(4):
                        kt = start * 64 + c * 128
                        S.op("pe", lambda e: e.matmul(PB[ps][:, c * 64:(c + 1) * 64], kn[hs, kt:kt + 128],
                                                      qn[hs, i * 64:(i + 1) * 64], start=True, stop=True),
                             reads=[r_kn, r_qn], writes=[RP[ps]], add=(c > 0))
                    S.op("dve", lambda e: e.tensor_tensor(scs[u][:], PB[ps][:, 0:256].rearrange("p (c q) -> p c q", q=64),
                                                          BT[:, h, pp0:pp0 + 7:2, :], ALU.add),
                         reads=[RP[ps], r_BT], writes=[r_scs[u]])
                    S.op("act", lambda e: e.activation(Eb[u][:], scs[u][:], AF.Exp), reads=[r_scs[u]], writes=[r_E[u]])

                def na_stage2(n):
                    hl, i = units[n]
                    hs = slice(hl * 64, (hl + 1) * 64)
                    start = min(max(i - 4, 0), 24)
                    a = start % 2
                    blk0 = start // 2
                    u = n % 3
                    v = n % 2
                    po = 6 + v
                    for c in range(4):
                        S.op("pe", lambda e: e.matmul(PB[po][0:64, 0:65], Eb[u][:, c, :], Vtm[a][:, blk0 + c, hl, :],
                                                      start=(c == 0), stop=(c == 3)),
                             reads=[r_E[u], r_V[a]], writes=[RP[po]], add=(c > 0))
                    S.op("dve", lambda e: e.reciprocal(rden[v][:], PB[po][0:64, 64:65]), reads=[RP[po]], writes=[r_rden[v]])
                    S.op("dve", lambda e: e.tensor_scalar(otm[:, i, hs], PB[po][0:64, 0:64], rden[v][:], None, ALU.mult),
                         reads=[RP[po], r_rden[v]], writes=[r_otm], add=(n > 0))

                na_stage1(0)
                na_stage1(1)
                for n in range(64):
                    if n + 2 < 64:
                        na_stage1(n + 2)
                    na_stage2(n)
                for i0 in range(0, 32, 8):
                    pb = (i0 // 8) % 2
                    for q in range(8):
                        S.op("pe", lambda e: e.matmul(PB[pb][:, q * 64:(q + 1) * 64], otm[:, i0 + q, :], identb[0:64, 0:64],
                                                      start=True, stop=True),
                             reads=[r_otm, r_idb], writes=[RP[pb]], add=(q > 0))
                    evac(ybT[:, i0 * 64:(i0 + 8) * 64], PB[pb][:], [RP[pb]], [r_ybT], add=(i0 > 0))
                S.dma("pool", yb_d[hp * 128:(hp + 1) * 128, t0:t0 + T], ybT[:], reads=[r_ybT])
        S.barrier()


def _prep(inputs):
    f = lambda a: np.ascontiguousarray(np.asarray(a, dtype=np.float32))
    I = {k: np.asarray(v) for k, v in inputs.items()}
    com = {}
    com["g1"] = f(I["norm1_g"][0][None, :])
    com["g2"] = f(I["norm2_g"][0][None, :])
    com["w_in"] = f(I["w_in"][0])
    com["mus"] = f(np.stack([I["mu_prev"][0].reshape(15, 128).T, I["mu_next"][0].reshape(15, 128).T], axis=1))
    v9 = np.stack([I["w0_f"][0], I["w0_b"][0], I["a0_f"][0], I["a0_b"][0], I["k_k"][0], I["k_a"][0],
                   I["r_k"][0].reshape(512), I["lnx_g"][0], I["lnx_b"][0]])
    com["vec"] = f(v9.reshape(9, 4, 128).transpose(2, 0, 1))
    com["lora"] = f(np.stack([np.concatenate([I["w_up_f"][0], I["w_up_b"][0]], 0),
                              np.concatenate([I["a_up_f"][0], I["a_up_b"][0]], 0), I["g_up"][0]], axis=1))
    com["qkg"] = f(np.stack([np.tile(I["q_gain"][0], 2), np.tile(I["k_gain"][0], 2)], axis=1))
    rpb = I["rpb"][0]
    kq = np.clip(np.arange(64)[:, None] - np.arange(64)[None, :] + 15, 0, 30)
    bt = np.zeros((2, 64, 8, 14, 64), np.float32)
    for r2 in range(2):
        for pp in range(14):
            bt[r2, :, :, pp, :] = rpb[:, pp + r2, :][:, kq].transpose(1, 0, 2)
    com["btab"] = f(bt.reshape(128, 8, 14, 64))
    cols = np.arange(64)
    cs = np.clip(cols - 8, 0, 48)
    cm = (cols[None, :] >= cs[:, None]) & (cols[None, :] < cs[:, None] + 16)
    m = np.where(cm.T, 0.0, NEG).astype(np.float32)
    com["nmask"] = f(np.concatenate([m, m], 0))
    com["bg"] = f(I["b_gate"][0].reshape(16, 128).T)
    com["w_oa"] = f(I["w_o_rwkv"][0])
    com["w_ob"] = f(I["w_o_na"][0])
    com["w_out"] = f(I["w_out"][0])
    com["wr"] = f(np.concatenate([I["w_router_group"][0], I["w_router_expert"][0]], 1))
    com["br"] = f(np.concatenate([I["b_router_group"][0], I["b_router_expert"][0]])[None, :])
    com["wge"] = f(I["w_gate_e"][0])
    com["wue"] = f(I["w_up_e"][0])
    com["wde"] = f(I["w_down_e"][0])
    com["ident"] = np.eye(128, dtype=np.float32)
    bo = np.zeros((128, 128), np.float32); bo[:64, :64] = 1; bo[64:, 64:] = 1
    com["bones"] = bo
    s_i = np.arange(64)[:, None]; t_i = np.arange(64)[None, :]
    rm = np.zeros((2, 64, 4, 2, 64), np.float32)
    for d in range(2):
        st_ = (s_i < t_i) if d == 0 else (s_i > t_i)
        inc = (s_i <= t_i) if d == 0 else (s_i >= t_i)
        rm[:, :, 0, d, :] = st_
        rm[:, :, 1, d, :] = inc
        rm[:, :, 2, d, :] = st_.T
        rm[:, :, 3, d, :] = 1.0
    rm = rm.reshape(128, 4, 2, 64)
    com["rmask"] = rm
    sc = np.ones((128, T), np.float32); sc[:, ::64] = 0
    com["scanm"] = sc
    xs = f(I["x"]).reshape(NCORES, NT, D)
    return com, xs


_CACHE = {}


def kernel(**inputs):
    com, xs = _prep(inputs)
    if "nc" not in _CACHE:
        _CACHE["nc"] = build2()
    nc = _CACHE["nc"]
    in_maps = [dict(com, x=xs[c]) for c in range(NCORES)]
    res = run_bass_kernel_spmd(nc, in_maps, core_ids=list(range(NCORES)))
    outs = [np.asarray(res.results[c]["out"], dtype=np.float32) for c in range(NCORES)]
    return np.stack(outs).reshape(16, T, D)
```

```python
import math
import numpy as np
from contextlib import ExitStack
import concourse.bass as bass
import concourse.mybir as mybir
from concourse.bass_utils import run_bass_kernel_spmd

F32 = mybir.dt.float32
BF16 = mybir.dt.bfloat16
AF = mybir.ActivationFunctionType
ALU = mybir.AluOpType
AX = mybir.AxisListType

NCORES = 8
T = 2048
NT = 4096
D = 1024
KAPPA = math.exp(-0.5)
NEG = -30000.0


class Res:
    __slots__ = ("name", "writers", "readers", "excl")

    def __init__(self, name="", excl=False):
        self.name = name
        self.writers = []
        self.readers = []
        self.excl = excl


class _PEProxy:
    def __init__(self, sched):
        self.s = sched

    def matmul(self, out, lhsT, rhs, **kw):
        s = self.s
        shp = list(lhsT.shape)
        k = shp[0]
        m = 1
        for d in shp[1:]:
            m *= d
        rnd = lambda v: 32 if v <= 32 else (64 if v <= 64 else 128)
        mode = (rnd(k), rnd(m), str(lhsT.dtype), lhsT.base_partition())
        if s.pe_mode is not None and mode != s.pe_mode and s.last_tok["pe"] is not None:
            s._emit_waits("pe", [s.last_tok["pe"]])
        s.pe_mode = mode
        return s.nc.tensor.matmul(out, lhsT, rhs, **kw)


class Sched:
    EPOCH = 6000

    def __init__(self, nc, stack, n_dma_sems=24, needed=None):
        self.nc = nc
        self.stack = stack
        self.needed_in = needed
        self.needed_out = set()
        self.incmap = {}
        self.inc_count = {}
        self.eng = {"pe": nc.tensor, "act": nc.scalar, "dve": nc.vector,
                    "pool": nc.gpsimd, "sp": nc.sync}
        self.cur_sem = {}
        self.cnt = {}
        self.seen = {e: {} for e in self.eng}
        self.sems = {}
        self.nsem = 0
        for e in self.eng:
            self._new_epoch(e)
        self.dma_keys = []
        self.dma_cnt = []
        for i in range(n_dma_sems):
            k = ("dma", i)
            self.sems[k] = stack.enter_context(nc.semaphore(f"dq{i}"))
            self.dma_keys.append(k)
            self.dma_cnt.append(0)
        self.dma_rr = 0
        self.dma_rr_sw = 0
        self.last_tok = {e: None for e in self.eng}
        self.all_dma_toks = {}
        self.n_inst = {e: 0 for e in self.eng}
        self.n_wait = 0
        self.pe_mode = None
        self.pe_proxy = _PEProxy(self)

    def _new_epoch(self, e):
        k = (e, self.nsem)
        self.nsem += 1
        self.sems[k] = self.stack.enter_context(self.nc.semaphore(f"e_{e}_{self.nsem}"))
        self.cur_sem[e] = k
        self.cnt[e] = 0

    def _emit_waits(self, e, deps):
        need = {}
        for tok in deps:
            if tok is None:
                continue
            k, v = tok
            if self.seen[e].get(k, 0) >= v:
                continue
            if need.get(k, 0) < v:
                need[k] = v
        eng = self.eng[e]
        for k, v in need.items():
            if k[0] == "dma":
                val = v
            else:
                self.needed_out.add((k, v))
                val = self.incmap[(k, v)] if self.needed_in is not None else v
            eng.wait_ge(self.sems[k], val)
            self.seen[e][k] = v
            self.n_wait += 1

    def _deps(self, reads, writes, e=None):
        deps = []
        other = lambda t: t[0][0] != e
        for r in reads:
            if e == "pe":
                deps.extend(t for t in r.writers if other(t))
            else:
                deps.extend(r.writers)
            if r.excl:
                deps.extend(t for t in r.readers if other(t))
        for w in writes:
            deps.extend(t for t in w.writers if other(t))
            deps.extend(t for t in w.readers if other(t))
        return deps

    def _commit(self, tok, reads, writes, add=False):
        for r in reads:
            r.readers.append(tok)
            if len(r.readers) > 48:
                best = {}
                for k, v in r.readers:
                    if best.get(k, 0) < v:
                        best[k] = v
                r.readers = list(best.items())
        for w in writes:
            if add:
                w.writers.append(tok)
            else:
                w.writers = [tok]
            w.readers = []

    def op(self, e, fn, reads=(), writes=(), add=False):
        if self.cnt[e] >= self.EPOCH:
            self._new_epoch(e)
        self._emit_waits(e, self._deps(reads, writes, e))
        ins = fn(self.pe_proxy if e == "pe" else self.eng[e])
        k = self.cur_sem[e]
        self.cnt[e] += 1
        tok = (k, self.cnt[e])
        if self.needed_in is None:
            ins.then_inc(self.sems[k], 1)
        elif tok in self.needed_in:
            ins.then_inc(self.sems[k], 1)
            self.inc_count[k] = self.inc_count.get(k, 0) + 1
            self.incmap[tok] = self.inc_count[k]
            self.n_inc = getattr(self, "n_inc", 0) + 1
        self.last_tok[e] = tok
        self.n_inst[e] += 1
        self._commit(tok, reads, writes, add)
        return tok

    def dma(self, e, out, in_, reads=(), writes=(), add=False, **kw):
        nsw = len(self.dma_keys) // 4
        if e == "pool":
            i = self.dma_rr_sw
            self.dma_rr_sw = (self.dma_rr_sw + 1) % nsw
        else:
            i = nsw + self.dma_rr
            self.dma_rr = (self.dma_rr + 1) % (len(self.dma_keys) - nsw)
        k = self.dma_keys[i]
        deps = self._deps(reads, writes, e)
        if self.dma_cnt[i] > 0:
            deps.append((k, 16 * self.dma_cnt[i]))
        self._emit_waits(e, deps)
        ins = self.eng[e].dma_start(out=out, in_=in_, **kw)
        ins.then_inc(self.sems[k], 16)
        self.dma_cnt[i] += 1
        tok = (k, 16 * self.dma_cnt[i])
        self.all_dma_toks[k] = tok
        self.n_inst[e] += 1
        self._commit(tok, reads, writes, add)
        return tok

    def barrier(self):
        toks = [t for t in self.last_tok.values() if t is not None]
        toks += list(self.all_dma_toks.values())
        for e in self.eng:
            self._emit_waits(e, toks)


def build(debug=False, phases="ABCDE", needed=None, info=None):
    nc = bass.Bass("TRN2", target_bir_lowering=False)

    def din(name, shape, dt=F32):
        return nc.dram_tensor(name, list(shape), dt, kind="ExternalInput").ap()

    skind = "ExternalOutput" if debug else "Internal"

    def dscr(name, shape, dt):
        return nc.dram_tensor(name, list(shape), dt, kind=skind).ap()

    x = din("x", [NT, D])
    g1 = din("g1", [1, D])
    g2 = din("g2", [1, D])
    w_in = din("w_in", [D, 5504])
    mus = din("mus", [128, 2, 15])
    vec = din("vec", [128, 9, 4])
    lora = din("lora", [128, 3, 512])
    qkg = din("qkg", [128, 2])
    btab = din("btab", [128, 8, 14, 64])
    nmask = din("nmask", [128, 64])
    bg = din("bg", [128, 16])
    w_oa = din("w_oa", [512, D])
    w_ob = din("w_ob", [512, D])
    w_out = din("w_out", [D, D])
    wr = din("wr", [D, 36])
    br = din("br", [1, 36])
    wge = din("wge", [32, D, 256])
    wue = din("wue", [32, D, 256])
    wde = din("wde", [32, 256, D])
    identd = din("ident", [128, 128])
    bonesd = din("bones", [128, 128])
    rmaskd = din("rmask", [128, 4, 2, 64])
    scand = din("scanm", [128, T])
    out = nc.dram_tensor("out", [NT, D], F32, kind="ExternalOutput").ap()

    hT_d = dscr("hT_d", [D, NT], BF16)
    p_d = dscr("p_d", [3456, NT], F32)
    ya_d = din("ya_d", [512, NT], BF16) if (debug and "B" not in phases) else dscr("ya_d", [512, NT], BF16)
    yb_d = dscr("yb_d", [512, NT], BF16)
    wgu_s = nc.dram_tensor("wgu_s", [32, 128, 8, 512], BF16, kind="Internal").ap()
    wdn_s = nc.dram_tensor("wdn_s", [32, 128, 2, D], BF16, kind="Internal").ap()
    xm_d = din("xm_d", [NT, D], F32) if (debug and "D" not in phases) else dscr("xm_d", [NT, D], F32)

    with ExitStack() as gst:
        S = Sched(nc, gst, n_dma_sems=32, needed=needed)
        PB = [gst.enter_context(nc.psum_tensor(f"pb{i}", [128, 512], F32)) for i in range(8)]
        RP = [Res(f"pb{i}", excl=True) for i in range(8)]

        def gsb(n, s, d):
            return gst.enter_context(nc.sbuf_tensor(n, s, d))

        identf = gsb("identf", [128, 128], F32); r_idf = Res()
        identb = gsb("identb", [128, 128], BF16); r_idb = Res()
        S.dma("sp", identf[:], identd, writes=[r_idf])
        S.op("dve", lambda e: e.tensor_copy(identb[:], identf[:]), reads=[r_idf], writes=[r_idb])

        rr = {"ev": 0}

        conv_jobs = []
        for e_ in range(32):
            conv_jobs.append((wgu_s[e_][:, :, 0:256], wge[e_].rearrange("(k p) n -> p k n", p=128)))
            conv_jobs.append((wgu_s[e_][:, :, 256:512], wue[e_].rearrange("(k p) n -> p k n", p=128)))
            conv_jobs.append((wdn_s[e_], wde[e_].rearrange("(c p) n -> p c n", p=128)))
        conv_jobs.reverse()

        def conv_step(n=1):
            while n > 0 and conv_jobs:
                o_, i_ = conv_jobs.pop()
                S.dma("pool", o_, i_)
                n -= 1

        def evac(out_ap, in_ap, reads, writes, add=False):
            rr["ev"] += 1
            if rr["ev"] % 2:
                return S.op("act", lambda e: e.copy(out_ap, in_ap), reads=reads, writes=writes, add=add)
            return S.op("dve", lambda e: e.tensor_copy(out_ap, in_ap), reads=reads, writes=writes, add=add)

        if "A" in phases:
            with ExitStack() as st:
                def sb(n, s, d):
                    return st.enter_context(nc.sbuf_tensor(n, s, d))
                wA = sb("wA", [128, 8, 3456], BF16); r_wA = Res()
                for k in range(8):
                    S.dma("pool", wA[:, k, :], w_in[k * 128:(k + 1) * 128, 0:3456], writes=[r_wA], add=True)
                g1t = sb("g1t", [128, D], F32); r_g1 = Res()
                S.dma("sp", g1t[:], g1.broadcast_to([128, D]), writes=[r_g1])
                xt = [sb(f"xt{i}", [128, 4, D], F32) for i in range(2)]; r_xt = [Res(), Res()]
                junk = sb("junk", [128, D], F32)
                ss = [sb(f"ss{i}", [128, 4], F32) for i in range(2)]; r_ss = [Res(), Res()]
                hb = sb("hb", [128, 4, D], BF16); r_hb = Res()
                hT = [sb(f"hT{i}", [128, 8, 512], BF16) for i in range(2)]; r_hT = [Res(), Res()]
                stg = [sb(f"stg{i}", [128, 512], F32) for i in range(4)]; r_stg = [Res() for _ in range(4)]
                for tb in range(8):
                    i = tb % 2
                    S.dma("sp", xt[i][:], x[tb * 512:(tb + 1) * 512, :].rearrange("(j p) d -> p j d", p=128),
                          writes=[r_xt[i]])
                    for j in range(4):
                        S.op("act", lambda e, j=j: e.activation(junk[:], xt[i][:, j, :], AF.Square,
                                                               accum_out=ss[i][:, j:j + 1]),
                             reads=[r_xt[i]], writes=[r_ss[i]], add=(j > 0))
                    S.op("act", lambda e: e.activation(ss[i][:], ss[i][:], AF.Sqrt, bias=1e-6, scale=1.0 / D),
                         reads=[r_ss[i]], writes=[r_ss[i]])
                    S.op("dve", lambda e: e.reciprocal(ss[i][:], ss[i][:]), reads=[r_ss[i]], writes=[r_ss[i]])
                    for j in range(4):
                        S.op("dve", lambda e, j=j: e.scalar_tensor_tensor(hb[:, j, :], xt[i][:, j, :], ss[i][:, j:j + 1],
                                                                         g1t[:], ALU.mult, ALU.mult),
                             reads=[r_xt[i], r_ss[i], r_g1], writes=[r_hb], add=(j > 0))
                    for k in range(8):
                        b = k % 4
                        for j in range(4):
                            S.op("pe", lambda e, j=j, k=k, b=b: e.matmul(PB[b][:, j * 128:(j + 1) * 128],
                                                                       hb[:, j, k * 128:(k + 1) * 128], identb[:],
                                                                       start=True, stop=True),
                                 reads=[r_hb, r_idb], writes=[RP[b]], add=(j > 0))
                        evac(hT[i][:, k, :], PB[b][:], [RP[b]], [r_hT[i]], add=(k > 0))
                    S.dma("pool", hT_d.rearrange("(k p) t -> p k t", p=128)[:, :, tb * 512:(tb + 1) * 512], hT[i][:],
                          reads=[r_hT[i]])
                    for cc in range(27):
                        b = 4 + cc % 4
                        for k in range(8):
                            S.op("pe", lambda e, k=k, cc=cc, b=b: e.matmul(PB[b][:], wA[:, k, cc * 128:(cc + 1) * 128],
                                                                         hT[i][:, k, :], start=(k == 0), stop=(k == 7)),
                                 reads=[r_wA, r_hT[i]], writes=[RP[b]], add=(k > 0))
                        q = cc % 4
                        evac(stg[q][:], PB[b][:], [RP[b]], [r_stg[q]])
                        S.dma("pool", p_d[cc * 128:(cc + 1) * 128, tb * 512:(tb + 1) * 512], stg[q][:], reads=[r_stg[q]])
                S.barrier()

        PHASE_B(nc, S, PB, RP, locals()) if "B" in phases else None
        conv_step(1000)
        PHASE_C(nc, S, PB, RP, locals()) if "C" in phases else None
        PHASE_D(nc, S, PB, RP, locals()) if "D" in phases else None
        PHASE_E(nc, S, PB, RP, locals()) if "E" in phases else None
        S.barrier()
        print("inst", S.n_inst, "waits", S.n_wait, "sems", S.nsem, "incs", getattr(S, "n_inc", None))
        if info is not None:
            info["needed"] = S.needed_out
    return nc


def build2(debug=False, phases="ABCDE"):
    info = {}
    build(debug=debug, phases=phases, info=info)
    return build(debug=debug, phases=phases, needed=info["needed"])


def PHASE_B(nc, S, PB, RP, G):
    p_d, ya_d, mus, vec, lora, bonesd, rmaskd, scand = (G[k] for k in ("p_d", "ya_d", "mus", "vec", "lora", "bonesd", "rmaskd", "scand"))
    identb, r_idb, identf, r_idf = G["identb"], G["r_idb"], G["identf"], G["r_idf"]
    GN_EPS = 64e-5
    with ExitStack() as st:
        def sb(n, s, d):
            return st.enter_context(nc.sbuf_tensor(n, s, d))
        mu = sb("mu", [128, 2, 15], F32); r_mu = Res()
        c0 = sb("c0", [128, 15], F32)
        vc = sb("vc", [128, 9, 4], F32); r_vc = Res()
        omka = sb("omka", [128, 4], F32)
        lwt = sb("lwt", [128, 3, 512], BF16); r_lwt = Res()
        bones = sb("bonesB", [128, 128], F32); r_bones = Res()
        RM = sb("RM", [128, 4, 2, 64], F32); r_RM = Res()
        scm = sb("scm", [128, T], BF16); r_scm = Res()
        S.dma("sp", mu[:], mus, writes=[r_mu])
        S.dma("sp", vc[:], vec, writes=[r_vc])
        S.dma("pool", lwt[:], lora, writes=[r_lwt])
        S.dma("sp", bones[:], bonesd, writes=[r_bones])
        S.dma("sp", RM[:], rmaskd, writes=[r_RM])
        S.dma("pool", scm[:], scand, writes=[r_scm])
        S.op("dve", lambda e: e.tensor_tensor(c0[:], mu[:, 0, :], mu[:, 1, :], ALU.add), reads=[r_mu], writes=[r_mu])
        S.op("dve", lambda e: e.tensor_scalar(c0[:], c0[:], -1.0, 1.0, ALU.mult, ALU.add), reads=[r_mu], writes=[r_mu])
        S.op("dve", lambda e: e.tensor_scalar(omka[:], vc[:, 5, :], -1.0, 1.0, ALU.mult, ALU.add), reads=[r_vc], writes=[r_vc])
        Pt = sb("Pt", [128, T + 2], F32); r_Pt = Res()
        S.op("pool", lambda e: e.memset(Pt[:], 0.0), writes=[r_Pt])
        Pt2 = sb("Pt2", [128, T + 2], F32); r_Pt2 = Res()
        S.op("pool", lambda e: e.memset(Pt2[:], 0.0), writes=[r_Pt2])
        slc = {"n": 0}
        L = [sb(f"L{i}", [128, T], BF16) for i in range(3)]; r_L = [Res() for _ in range(3)]
        names = ["r_s", "k_s", "v_s", "a_f", "a_b", "sg", "kkn", "t1", "t2", "bonus"]
        Tt = {n: sb("B_" + n, [128, T], F32) for n in names}
        Rt = {n: Res(n) for n in names}
        HAT = [sb(f"HAT{d}", [128, 32, 4, 64], BF16) for d in range(2)]; r_HAT = [Res(), Res()]
        ytm = sb("ytm", [128, 32, 64], F32); r_ytm = Res()
        yo = sb("yo", [128, T], BF16); r_yo = Res()
        vb = sb("vbB", [128, T], BF16); r_vb = Res()
        PCs = [sb(f"PCs{d}", [128, 32], F32) for d in range(2)]; r_PCs = [Res(), Res()]
        totb = sb("totb", [128, 32], F32); r_totb = Res()
        ytmp = sb("ytmp", [128, 128], F32); r_ytmp = Res()
        gs = sb("gs", [128, 4, 32], F32); r_gs = Res()

        def ztile(n, shp, dt):
            t_ = sb(n, shp, dt)
            S.op("pool", lambda e: e.memset(t_[:], 0.0))
            return t_
        Nbd = [ztile(f"Nbd{i}", [128, 2, 128], BF16) for i in range(2)]; r_Nbd = [Res(), Res()]
        Abd = [ztile(f"Abd{i}", [128, 2, 128], BF16) for i in range(2)]; r_Abd = [Res(), Res()]
        Akabd = [ztile(f"Akabd{i}", [128, 2, 128], BF16) for i in range(2)]; r_Akabd = [Res(), Res()]
        ATbd = [ztile(f"ATbd{i}", [128, 2, 128], BF16) for i in range(2)]; r_ATbd = [Res(), Res()]
        KD = [ztile(f"KD{i}", [128, 2, 128], BF16) for i in range(2)]; r_KD = [Res(), Res()]
        Nst = [ztile(f"Nst{i}", [128, 2, 128], BF16) for i in range(2)]; r_Nst = [Res(), Res()]
        Ast = [ztile(f"Ast{i}", [128, 2, 128], BF16) for i in range(2)]; r_Ast = [Res(), Res()]
        Xbs = [sb(f"Xb{i}", [128, 2, 128], BF16) for i in range(2)]; r_Xbs = [Res(), Res()]
        T0 = sb("T0", [128, 256], BF16); r_T0 = Res()
        T1 = sb("T1", [128, 256], BF16); r_T1 = Res()
        Vst = [sb(f"Vst{i}", [128, 2, 64], BF16) for i in range(2)]; r_Vst = [Res(), Res()]
        QM1bd = ztile("QM1bd", [128, 2, 2, 128], BF16); r_QM1bd = Res()
        QM2bd = ztile("QM2bd", [128, 2, 2, 128], BF16); r_QM2bd = Res()
        STs = sb("STs", [128, 2, 64], BF16); r_STs = Res()
        Icst = sb("Icst", [128, 2, 2, 64], F32); I2b = sb("I2b", [128, 64], BF16); MQ2 = sb("MQ2", [128, 2, 2, 64], F32); r_cst = Res()
        S.op("pool", lambda e: e.memset(Icst[:], 0.0), writes=[r_cst])
        for d_ in range(2):
            S.op("dve", lambda e: e.tensor_tensor(Icst[:, d_, 1, :], identf[:, 0:64], identf[:, 64:128], ALU.add), reads=[r_idf, r_cst], writes=[r_cst])
        S.op("dve", lambda e: e.tensor_copy(I2b[:], Icst[:, 0, 1, :]), reads=[r_cst], writes=[r_cst])
        S.op("dve", lambda e: e.tensor_copy(MQ2[:, :, 0, :], RM[:, 1]), reads=[r_RM, r_cst], writes=[r_cst])
        S.op("dve", lambda e: e.tensor_copy(MQ2[:, :, 1, :], RM[:, 3]), reads=[r_RM, r_cst], writes=[r_cst])
        S.barrier()

        def v3(ap):
            return ap.rearrange("p (c t) -> p c t", t=64)

        def shift_load(fc, s_, dst, r_dst):
            slc["n"] += 1
            Pb, r_Pb = (Pt, r_Pt) if slc["n"] % 2 else (Pt2, r_Pt2)
            S.dma("sp", Pb[:, 1:T + 1], p_d[fc * 128:(fc + 1) * 128, s_ * T:(s_ + 1) * T], writes=[r_Pb])
            S.op("act", lambda e: e.mul(dst[:], Pb[:, 1:T + 1], c0[:, fc:fc + 1]), reads=[r_Pb, r_mu], writes=[r_dst])
            S.op("dve", lambda e: e.scalar_tensor_tensor(dst[:], Pb[:, 0:T], mu[:, 0, fc:fc + 1], dst[:], ALU.mult, ALU.add),
                 reads=[r_Pb, r_mu, r_dst], writes=[r_dst])
            S.op("dve", lambda e: e.scalar_tensor_tensor(dst[:], Pb[:, 2:T + 2], mu[:, 1, fc:fc + 1], dst[:], ALU.mult, ALU.add),
                 reads=[r_Pb, r_mu, r_dst], writes=[r_dst])

        def mm_sig(dst, r_dst, li, rows, bias_ap, hp, func):
            for c in range(4):
                b = c % 2
                S.op("pe", lambda e: e.matmul(PB[b][:], lwt[rows, li, hp * 128:(hp + 1) * 128], L[li][rows, c * 512:(c + 1) * 512],
                                              start=True, stop=True), reads=[r_lwt, r_L[li]], writes=[RP[b]])
                if func is None:
                    S.op("act", lambda e: e.copy(dst[:, c * 512:(c + 1) * 512], PB[b][:]), reads=[RP[b]], writes=[r_dst], add=(c > 0))
                else:
                    S.op("act", lambda e: e.activation(dst[:, c * 512:(c + 1) * 512], PB[b][:], func, bias=bias_ap),
                         reads=[RP[b], r_vc], writes=[r_dst], add=(c > 0))

        def bones_mm(src, r_src, post):
            for c in range(4):
                b = c % 2
                S.op("pe", lambda e: e.matmul(PB[b][:], bones[:], src[:, c * 512:(c + 1) * 512], start=True, stop=True),
                     reads=[r_bones, r_src], writes=[RP[b]])
                post(c, b)

        r_s, k_s, v_s, a_f, a_b, sg, kkn, t1, t2, bonus = (Tt[n] for n in names)
        X = Pt[:, 1:T + 1]
        for s_ in range(2):
            shift_load(12, s_, t1, Rt["t1"])
            S.op("act", lambda e: e.activation(L[0][:], t1[:], AF.Tanh), reads=[Rt["t1"]], writes=[r_L[0]])
            shift_load(13, s_, t1, Rt["t1"])
            S.op("act", lambda e: e.copy(L[1][:], t1[:]), reads=[Rt["t1"]], writes=[r_L[1]])
            shift_load(14, s_, t1, Rt["t1"])
            S.op("act", lambda e: e.activation(L[2][:], t1[:], AF.Sigmoid), reads=[Rt["t1"]], writes=[r_L[2]])
            for hp in range(4):
                shift_load(hp, s_, r_s, Rt["r_s"])
                shift_load(4 + hp, s_, k_s, Rt["k_s"])
                shift_load(8 + hp, s_, v_s, Rt["v_s"])
                lo, hi = slice(0, 64), slice(64, 128)
                mm_sig(a_f, Rt["a_f"], 1, lo, vc[:, 2, hp:hp + 1], hp, AF.Sigmoid)
                mm_sig(a_b, Rt["a_b"], 1, hi, vc[:, 3, hp:hp + 1], hp, AF.Sigmoid)
                S.op("act", lambda e: e.mul(t1[:], k_s[:], vc[:, 4, hp:hp + 1]), reads=[Rt["k_s"], r_vc], writes=[Rt["t1"]])
                S.op("act", lambda e: e.activation(t2[:], t1[:], AF.Square), reads=[Rt["t1"]], writes=[Rt["t2"]])
                bones_mm(t2, Rt["t2"], lambda c, b: S.op(
                    "act", lambda e: e.activation(kkn[:, c * 512:(c + 1) * 512], PB[b][:], AF.Sqrt, bias=1e-12, scale=1.0),
                    reads=[RP[b]], writes=[Rt["kkn"]], add=(c > 0)))
                S.op("dve", lambda e: e.reciprocal(kkn[:], kkn[:]), reads=[Rt["kkn"]], writes=[Rt["kkn"]])
                S.op("dve", lambda e: e.tensor_tensor(kkn[:], kkn[:], t1[:], ALU.mult), reads=[Rt["kkn"], Rt["t1"]], writes=[Rt["kkn"]])
                S.op("dve", lambda e: e.tensor_scalar(t2[:], a_f[:], vc[:, 5, hp:hp + 1], omka[:, hp:hp + 1], ALU.mult, ALU.add),
                     reads=[Rt["a_f"], r_vc], writes=[Rt["t2"]])
                S.op("dve", lambda e: e.tensor_tensor(t2[:], t2[:], k_s[:], ALU.mult), reads=[Rt["t2"], Rt["k_s"]], writes=[Rt["t2"]])
                S.op("dve", lambda e: e.tensor_scalar(t1[:], a_b[:], vc[:, 5, hp:hp + 1], omka[:, hp:hp + 1], ALU.mult, ALU.add),
                     reads=[Rt["a_b"], r_vc], writes=[Rt["t1"]])
                S.op("dve", lambda e: e.tensor_tensor(k_s[:], t1[:], k_s[:], ALU.mult), reads=[Rt["t1"], Rt["k_s"]], writes=[Rt["k_s"]])
                S.op("dve", lambda e: e.tensor_tensor(t1[:], t2[:], k_s[:], ALU.add), reads=[Rt["t2"], Rt["k_s"]], writes=[Rt["t1"]])
                S.op("dve", lambda e: e.scalar_tensor_tensor(t1[:], t1[:], vc[:, 6, hp:hp + 1], r_s[:], ALU.mult, ALU.mult),
                     reads=[Rt["t1"], r_vc, Rt["r_s"]], writes=[Rt["t1"]])
                bones_mm(t1, Rt["t1"], lambda c, b: S.op(
                    "dve", lambda e: e.tensor_tensor(bonus[:, c * 512:(c + 1) * 512], PB[b][:], v_s[:, c * 512:(c + 1) * 512], ALU.mult),
                    reads=[RP[b], Rt["v_s"]], writes=[Rt["bonus"]], add=(c > 0)))
                for d in range(2):
                    a_d, r_ad = (a_f, Rt["a_f"]) if d == 0 else (a_b, Rt["a_b"])
                    kd, r_kd = (t2, Rt["t2"]) if d == 0 else (k_s, Rt["k_s"])
                    mm_sig(sg, Rt["sg"], 0, lo if d == 0 else hi, vc[:, d, hp:hp + 1], hp, AF.Sigmoid)
                    S.op("dve", lambda e: e.tensor_tensor_scan(t1[:], scm[:], sg[:], 0.0, ALU.mult, ALU.add),
                         reads=[r_scm, Rt["sg"]], writes=[Rt["t1"]])
                    S.op("dve", lambda e: e.tensor_copy(totb[:].unsqueeze(2), v3(t1[:])[:, :, 63:64]), reads=[Rt["t1"]], writes=[r_totb])
                    if d == 1:
                        S.op("dve", lambda e: e.tensor_tensor(t1[:], sg[:], t1[:], ALU.subtract), reads=[Rt["sg"], Rt["t1"]], writes=[Rt["t1"]])
                        S.op("dve", lambda e: e.tensor_tensor(v3(t1[:]), v3(t1[:]), totb[:].unsqueeze(2).broadcast_to([128, 32, 64]), ALU.add),
                             reads=[Rt["t1"], r_totb], writes=[Rt["t1"]])
                    S.op("act", lambda e: e.activation(PCs[d][:], totb[:], AF.Exp, scale=-KAPPA), reads=[r_totb], writes=[r_PCs[d]])
                    S.op("dve", lambda e: e.tensor_tensor(sg[:], t1[:], sg[:], ALU.subtract), reads=[Rt["sg"], Rt["t1"]], writes=[Rt["sg"]])
                    S.op("act", lambda e: e.activation(sg[:], sg[:], AF.Exp, scale=-KAPPA), reads=[Rt["sg"]], writes=[Rt["sg"]])
                    S.op("act", lambda e: e.activation(X, t1[:], AF.Exp, scale=KAPPA), reads=[Rt["t1"]], writes=[r_Pt])
                    S.op("act", lambda e: e.activation(t1[:], t1[:], AF.Exp, scale=-KAPPA), reads=[Rt["t1"]], writes=[Rt["t1"]])
                    S.op("dve", lambda e: e.tensor_tensor(a_d[:], a_d[:], kkn[:], ALU.mult), reads=[r_ad, Rt["kkn"]], writes=[r_ad])
                    S.op("dve", lambda e: e.tensor_tensor(HAT[d][:, :, 0, :], v3(a_d[:]), v3(X), ALU.mult),
                         reads=[r_ad, r_Pt], writes=[r_HAT[d]])
                    S.op("dve", lambda e: e.tensor_tensor(HAT[d][:, :, 1, :], v3(kd[:]), v3(X), ALU.mult),
                         reads=[r_kd, r_Pt], writes=[r_HAT[d]], add=True)
                    S.op("dve", lambda e: e.scalar_tensor_tensor(HAT[d][:, :, 2, :], v3(kkn[:]), -1.0, v3(sg[:]), ALU.mult, ALU.mult),
                         reads=[Rt["kkn"], Rt["sg"]], writes=[r_HAT[d]], add=True)
                    S.op("dve", lambda e: e.tensor_tensor(HAT[d][:, :, 3, :], v3(r_s[:]), v3(t1[:]), ALU.mult),
                         reads=[Rt["r_s"], Rt["t1"]], writes=[r_HAT[d]], add=True)
                S.op("pool", lambda e: e.memset(STs[:], 0.0), writes=[r_STs])
                S.op("pool", lambda e: e.memset(ytm[:], 0.0), writes=[r_ytm])
                S.op("act", lambda e: e.copy(vb[:], v_s[:]), reads=[Rt["v_s"]], writes=[r_vb])
                mm_sig(r_s, Rt["r_s"], 2, slice(0, 128), None, hp, None)
                HS = (slice(0, 64), slice(64, 128))
                UN = ((0, 0), (1, 0), (0, 1), (1, 1))

                def v2(ap, n=128):
                    return ap.rearrange("p (d x) -> p d x", x=n)

                def pre_pe(i):
                    cidx = (i, 31 - i)
                    G["conv_step"](1)
                    first = True
                    for d, hl in UN:
                        hs = HS[hl]
                        ci = cidx[d]
                        S.op("pe", lambda e: e.matmul(PB[0][hs, 256 + d * 128 + hl * 64:256 + d * 128 + hl * 64 + 64], HAT[d][hs, ci, 2, :], identb[hs, hs], start=True, stop=True),
                             reads=[r_HAT[d], r_idb], writes=[RP[0]], add=not first)
                        S.op("pe", lambda e: e.matmul(PB[1][hs, 256 + d * 64:256 + d * 64 + 64], HAT[d][hs, ci, 0, :], identb[hs, hs], start=True, stop=True),
                             reads=[r_HAT[d], r_idb], writes=[RP[1]], add=not first)
                        S.op("pe", lambda e: e.matmul(PB[1][hs, 384 + d * 64:384 + d * 64 + 64], vb[hs, ci * 64:(ci + 1) * 64], identb[hs, hs], start=True, stop=True),
                             reads=[r_vb, r_idb], writes=[RP[1]], add=True)
                        S.op("pe", lambda e: e.matmul(PB[0][hs, d * 128:(d + 1) * 128], HAT[d][hs, ci, 0, :],
                                                      HAT[d][hs, ci, 2:4, :].rearrange("p a t -> p (a t)"), start=True, stop=True),
                             reads=[r_HAT[d]], writes=[RP[0]], add=True)
                        S.op("pe", lambda e: e.matmul(PB[1][hs, d * 128:(d + 1) * 128], HAT[d][hs, ci, 2, :],
                                                      HAT[d][hs, ci, 0:2, :].rearrange("p a t -> p (a t)"), start=True, stop=True),
                             reads=[r_HAT[d]], writes=[RP[1]], add=True)
                        first = False

                def pre_ev(i):
                    p = i % 2
                    cidx = (i, 31 - i)
                    P3 = v2(PB[0][:, 256:512])
                    T0v = v2(T0[:])
                    T1v = v2(T1[:])
                    S.op("act", lambda e: e.copy(T0[:], PB[0][:, 0:256]), reads=[RP[0]], writes=[r_T0])
                    for hl in range(2):
                        hs = HS[hl]
                        cs = slice(hl * 64, (hl + 1) * 64)
                        S.op("act", lambda e: e.copy(ATbd[p][hs, :, cs], P3[hs, :, cs]), reads=[RP[0]], writes=[r_ATbd[p]], add=(hl > 0))
                    S.op("act", lambda e: e.copy(T1[:], PB[1][:, 0:256]), reads=[RP[1]], writes=[r_T1])
                    S.op("act", lambda e: e.copy(Xbs[p][:, :, 64:128], v2(PB[1][:, 256:384], 64)), reads=[RP[1]], writes=[r_Xbs[p]])
                    S.op("act", lambda e: e.copy(Vst[p][:], v2(PB[1][:, 384:512], 64)), reads=[RP[1]], writes=[r_Vst[p]])
                    S.op("pool", lambda e: e.tensor_tensor(Xbs[p][:, :, 0:64], T0v[:, :, 64:128], RM[:, 1], ALU.mult), reads=[r_T0, r_RM], writes=[r_Xbs[p]], add=True)
                    for hl in range(2):
                        hs = HS[hl]
                        cs = slice(hl * 64, (hl + 1) * 64)
                        S.op("pool", lambda e: e.tensor_tensor(Nst[p][hs, :, cs], T1v[hs, :, 0:64], RM[hs, 2], ALU.mult), reads=[r_T1, r_RM], writes=[r_Nst[p]], add=(hl > 0))
                    for hl in range(2):
                        hs = HS[hl]
                        cs = slice(hl * 64, (hl + 1) * 64)
                        S.op("pool", lambda e: e.tensor_tensor(Ast[p][hs, :, cs], T0v[hs, :, 0:64], RM[hs, 0], ALU.mult), reads=[r_T0, r_RM], writes=[r_Ast[p]], add=(hl > 0))
                    for hl in range(2):
                        hs = HS[hl]
                        cs = slice(hl * 64, (hl + 1) * 64)
                        S.op("pool", lambda e: e.tensor_tensor(Akabd[p][hs, :, cs], T1v[hs, :, 64:128], RM[hs, 2], ALU.mult), reads=[r_T1, r_RM], writes=[r_Akabd[p]], add=(hl > 0))
                    first = True
                    for d, hl in UN:
                        hs = HS[hl]
                        S.op("pool", lambda e: e.tensor_copy(KD[p][hs, d, hl * 64:(hl + 1) * 64], HAT[d][hs, cidx[d], 1, :]),
                             reads=[r_HAT[d]], writes=[r_KD[p]], add=not first)
                        first = False

                def seed(i):
                    p = i % 2
                    S.op("pe", lambda e: e.matmul(PB[2][:, 0:256], identb[:], Xbs[p][:].rearrange("p d x -> p (d x)"), start=True, stop=False),
                         reads=[r_idb, r_Xbs[p]], writes=[RP[2]])

                def level_pe(i, k):
                    p = i % 2
                    q = k % 2
                    if k == 0:
                        Nq, r_Nq, Aq, r_Aq = Nst[p], r_Nst[p], Ast[p], r_Ast[p]
                    else:
                        Nq, r_Nq, Aq, r_Aq = Nbd[q], r_Nbd[q], Abd[q], r_Abd[q]
                    if k < 5:
                        for d in range(2):
                            S.op("pe", lambda e: e.matmul(PB[3][:, d * 128:(d + 1) * 128], Aq[:, d, :], Nq[:, d, :], start=True, stop=True),
                                 reads=[r_Nq, r_Aq], writes=[RP[3]], add=(d > 0))
                    for d in range(2):
                        S.op("pe", lambda e: e.matmul(PB[2][:, d * 128:(d + 1) * 128], Nq[:, d, :], Xbs[p][:, d, :], start=False, stop=(k == 5)),
                             reads=[r_Nq, r_Xbs[p]], writes=[RP[2]], add=True)
                    if k < 4:
                        for d in range(2):
                            S.op("pe", lambda e: e.matmul(PB[4][:, d * 128:(d + 1) * 128], Nq[:, d, :], Aq[:, d, :], start=True, stop=True),
                                 reads=[r_Nq, r_Aq], writes=[RP[4]], add=(d > 0))

                def level_ev(i, k):
                    p = i % 2
                    q = (k + 1) % 2
                    if k < 5:
                        S.op("dve", lambda e: e.tensor_copy(Nbd[q][:].rearrange("p d x -> p (d x)"), PB[3][:, 0:256]), reads=[RP[3]], writes=[r_Nbd[q]])
                    S.op("act", lambda e: e.copy(Xbs[p][:].rearrange("p d x -> p (d x)"), PB[2][:, 0:256]), reads=[RP[2]], writes=[r_Xbs[p]])
                    if k < 4:
                        S.op("act", lambda e: e.copy(Abd[q][:].rearrange("p d x -> p (d x)"), PB[4][:, 0:256]), reads=[RP[4]], writes=[r_Abd[q]])

                def post1_pe(i):
                    p = i % 2
                    cidx = (i, 31 - i)
                    for d in range(2):
                        ci = cidx[d]
                        o5 = PB[5][:, d * 128:(d + 1) * 128]
                        o6 = PB[6][:, d * 128:(d + 1) * 128]
                        S.op("pe", lambda e: e.matmul(o5, ATbd[p][:, d, :], Xbs[p][:, d, :], start=True, stop=False),
                             reads=[r_ATbd[p], r_Xbs[p]], writes=[RP[5]], add=(d > 0))
                        S.op("pe", lambda e: e.matmul(o5[:, 0:64], identb[:], HAT[d][:, ci, 3, :], start=False, stop=False),
                             reads=[r_idb, r_HAT[d]], writes=[RP[5]], add=True)
                        S.op("pe", lambda e: e.matmul(o5[:, 64:128], identb[:], I2b[:], start=False, stop=True),
                             reads=[r_idb, r_cst], writes=[RP[5]], add=True)
                        S.op("pe", lambda e: e.matmul(o6, Akabd[p][:, d, :], Xbs[p][:, d, :], start=True, stop=False),
                             reads=[r_Akabd[p], r_Xbs[p]], writes=[RP[6]], add=(d > 0))
                        S.op("pe", lambda e: e.matmul(o6[:, 0:64], KD[p][:, d, :], HAT[d][:, ci, 3, :], start=False, stop=False),
                             reads=[r_KD[p], r_HAT[d]], writes=[RP[6]], add=True)
                        S.op("pe", lambda e: e.matmul(o6[:, 64:128], KD[p][:, d, :], I2b[:], start=False, stop=True),
                             reads=[r_KD[p], r_cst], writes=[RP[6]], add=True)

                def post1_ev(i):
                    P5 = PB[5][:, 0:256].rearrange("p (d a x) -> p d a x", a=2, x=64)
                    P6 = PB[6][:, 0:256].rearrange("p (d a x) -> p d a x", a=2, x=64)
                    for hl in range(2):
                        hs = HS[hl]
                        cs = slice(hl * 64, (hl + 1) * 64)
                        S.op("act", lambda e: e.copy(QM1bd[hs, :, :, cs], P5[hs]), reads=[RP[5]], writes=[r_QM1bd], add=(hl > 0))
                    for hl in range(2):
                        hs = HS[hl]
                        cs = slice(hl * 64, (hl + 1) * 64)
                        S.op("dve", lambda e: e.tensor_tensor(QM2bd[hs, :, :, cs], P6[hs], MQ2[hs], ALU.mult), reads=[RP[6], r_cst], writes=[r_QM2bd], add=(hl > 0))

                def scan_pe(i):
                    p = i % 2
                    for d in range(2):
                        for a_ in range(2):
                            o_ = PB[7][:, a_ * 128 + d * 64:a_ * 128 + (d + 1) * 64]
                            S.op("pe", lambda e: e.matmul(o_, QM1bd[:, d, a_, :], STs[:, d, :], start=True, stop=False),
                                 reads=[r_QM1bd, r_STs], writes=[RP[7]], add=not (d == 0 and a_ == 0))
                            S.op("pe", lambda e: e.matmul(o_, QM2bd[:, d, a_, :], Vst[p][:, d, :], start=False, stop=True),
                                 reads=[r_QM2bd, r_Vst[p]], writes=[RP[7]], add=True)

                def scan_ev(i):
                    cidx = (i, 31 - i)
                    S.op("dve", lambda e: e.tensor_copy(ytmp[:], PB[7][:, 0:128]), reads=[RP[7]], writes=[r_ytmp])
                    for d in range(2):
                        ci = cidx[d]
                        S.op("act", lambda e: e.mul(STs[:, d, :], PB[7][:, 128 + d * 64:128 + (d + 1) * 64], PCs[d][:, ci:ci + 1]),
                             reads=[RP[7], r_PCs[d]], writes=[r_STs], add=(d > 0))
                    for d in range(2):
                        ci = cidx[d]
                        S.op("pool", lambda e: e.tensor_tensor(ytm[:, ci, :], ytmp[:, d * 64:(d + 1) * 64], ytm[:, ci, :], ALU.add),
                             reads=[r_ytmp, r_ytm], writes=[r_ytm])

                pre_pe(0)
                pre_ev(0)
                seed(0)
                pre_pe(1)
                pre_ev(1)
                for k in range(6):
                    level_pe(0, k)
                    level_ev(0, k)
                seed(1)
                for i in range(32):
                    c = i + 1
                    has_c = c < 32
                    has_n = c + 1 < 32
                    if has_c:
                        level_pe(c, 0)
                    post1_pe(i)
                    if has_c:
                        level_ev(c, 0)
                    post1_ev(i)
                    if has_c:
                        level_pe(c, 1)
                    scan_pe(i)
                    if has_c:
                        level_ev(c, 1)
                    scan_ev(i)
                    if has_c:
                        level_pe(c, 2)
                    if has_n:
                        pre_pe(c + 1)
                    if has_c:
                        level_ev(c, 2)
                    if has_n:
                        pre_ev(c + 1)
                    if has_c:
                        for k in range(3, 6):
                            level_pe(c, k)
                            level_ev(c, k)
                    if has_n:
                        seed(c + 1)
                sq = t2[:].rearrange("p (g f) -> p g f", f=64)
                bc = lambda ap: ap.unsqueeze(2).broadcast_to([128, 32, 64])
                Vg = lambda f: S.op("dve", f, reads=[r_ytm, r_gs, Rt["t2"]], writes=[r_ytm, r_gs])
                Vg(lambda e: e.tensor_reduce(gs[:, 0, :], ytm[:], AX.X, ALU.add))
                Vg(lambda e: e.tensor_scalar(gs[:, 0, :], gs[:, 0, :], 1.0 / 64, None, ALU.mult))
                Vg(lambda e: e.tensor_tensor(ytm[:], ytm[:], bc(gs[:, 0, :]), ALU.subtract))
                S.op("act", lambda e: e.activation(sq, ytm[:], AF.Square), reads=[r_ytm], writes=[Rt["t2"]])
                Vg(lambda e: e.tensor_reduce(gs[:, 1, :], sq, AX.X, ALU.add))
                S.op("act", lambda e: e.activation(gs[:, 2, :], gs[:, 1, :], AF.Sqrt, bias=GN_EPS, scale=1.0 / 64), reads=[r_gs], writes=[r_gs])
                Vg(lambda e: e.reciprocal(gs[:, 2, :], gs[:, 2, :]))
                Vg(lambda e: e.tensor_tensor(ytm[:], ytm[:], bc(gs[:, 2, :]), ALU.mult))
                for c0_ in range(0, 32, 8):
                    b = (c0_ // 8) % 2
                    first = True
                    for hl in range(2):
                        hs = HS[hl]
                        for q in range(8):
                            S.op("pe", lambda e: e.matmul(PB[b][hs, q * 64:(q + 1) * 64], ytm[hs, c0_ + q, :], identf[hs, hs], start=True, stop=True),
                                 reads=[r_ytm, r_idf], writes=[RP[b]], add=not first)
                            first = False
                    S.op("act", lambda e: e.activation(t1[:, c0_ * 64:(c0_ + 8) * 64], PB[b][:], AF.Identity,
                                                       bias=vc[:, 8, hp:hp + 1], scale=vc[:, 7, hp:hp + 1]),
                         reads=[RP[b], r_vc], writes=[Rt["t1"]], add=(c0_ > 0))
                S.op("dve", lambda e: e.tensor_tensor(t1[:], t1[:], bonus[:], ALU.add), reads=[Rt["t1"], Rt["bonus"]], writes=[Rt["t1"]])
                S.op("dve", lambda e: e.tensor_tensor(yo[:], t1[:], r_s[:], ALU.mult), reads=[Rt["t1"], Rt["r_s"]], writes=[r_yo])
                S.dma("pool", ya_d[hp * 128:(hp + 1) * 128, s_ * T:(s_ + 1) * T], yo[:], reads=[r_yo])
        S.barrier()


def PHASE_D(nc, S, PB, RP, G):
    x, w_in, w_oa, w_ob, w_out, bg = G["x"], G["w_in"], G["w_oa"], G["w_ob"], G["w_out"], G["bg"]
    hT_d, ya_d, yb_d, xm_d, evac = G["hT_d"], G["ya_d"], G["yb_d"], G["xm_d"], G["evac"]
    with ExitStack() as st:
        def sb(n, s, d):
            return st.enter_context(nc.sbuf_tensor(n, s, d))
        woa = sb("woa", [128, 4, D], BF16); r_woa = Res()
        wob = sb("wob", [128, 4, D], BF16); r_wob = Res()
        wg = sb("wg", [128, 8, 2048], BF16); r_wg = Res()
        wo = sb("wo", [128, 8, D], BF16); r_wo = Res()
        bgt = sb("bgt", [128, 16], F32); r_bg = Res()
        S.dma("sp", bgt[:], bg, writes=[r_bg])
        S.dma("pool", woa[:], w_oa.rearrange("(k p) n -> p k n", p=128), writes=[r_woa])
        S.dma("pool", wob[:], w_ob.rearrange("(k p) n -> p k n", p=128), writes=[r_wob])
        for k in range(8):
            S.dma("pool", wg[:, k, :], w_in[k * 128:(k + 1) * 128, 3456:5504], writes=[r_wg], add=True)
            S.dma("pool", wo[:, k, :], w_out[k * 128:(k + 1) * 128, :], writes=[r_wo], add=True)
        ra = sb("ra", [128, 4, 512], BF16); r_ra = Res()
        rb = sb("rb", [128, 4, 512], BF16); r_rb = Res()
        hTb = sb("hTb", [128, 8, 512], BF16); r_hTb = Res()
        xt = sb("xtD", [128, 4, D], F32); r_xt = Res()
        xm = sb("xmD", [128, 4, D], F32); r_xm = Res()
        zT = sb("zT", [128, 8, 512], BF16); r_zT = [Res() for _ in range(8)]
        sga = [sb(f"sga{i}", [128, 512], F32) for i in range(2)]; r_sga = [Res(), Res()]
        sgb = [sb(f"sgb{i}", [128, 512], F32) for i in range(2)]; r_sgb = [Res(), Res()]
        z1 = [sb(f"z1{i}", [128, 512], F32) for i in range(2)]; r_z1 = [Res(), Res()]
        z2 = [sb(f"z2{i}", [128, 512], F32) for i in range(2)]; r_z2 = [Res(), Res()]
        for tb in range(8):
            tsl = slice(tb * 512, (tb + 1) * 512)
            S.dma("sp", ra[:], ya_d.rearrange("(k p) t -> p k t", p=128)[:, :, tsl], writes=[r_ra])
            S.dma("sp", rb[:], yb_d.rearrange("(k p) t -> p k t", p=128)[:, :, tsl], writes=[r_rb])
            S.dma("sp", hTb[:], hT_d.rearrange("(k p) t -> p k t", p=128)[:, :, tsl], writes=[r_hTb])
            S.dma("sp", xt[:], x[tsl, :].rearrange("(j p) d -> p j d", p=128), writes=[r_xt])
            for fc in range(8):
                i = fc % 2
                b0 = 4 * i
                fsl = slice(fc * 128, (fc + 1) * 128)
                for k in range(4):
                    S.op("pe", lambda e: e.matmul(PB[b0][:], woa[:, k, fsl], ra[:, k, :], start=(k == 0), stop=(k == 3)),
                         reads=[r_woa, r_ra], writes=[RP[b0]], add=(k > 0))
                for k in range(4):
                    S.op("pe", lambda e: e.matmul(PB[b0 + 1][:], wob[:, k, fsl], rb[:, k, :], start=(k == 0), stop=(k == 3)),
                         reads=[r_wob, r_rb], writes=[RP[b0 + 1]], add=(k > 0))
                for k in range(8):
                    S.op("pe", lambda e: e.matmul(PB[b0 + 2][:], wg[:, k, fsl], hTb[:, k, :], start=(k == 0), stop=(k == 7)),
                         reads=[r_wg, r_hTb], writes=[RP[b0 + 2]], add=(k > 0))
                for k in range(8):
                    S.op("pe", lambda e: e.matmul(PB[b0 + 3][:], wg[:, k, 1024 + fc * 128:1024 + (fc + 1) * 128], hTb[:, k, :],
                                                  start=(k == 0), stop=(k == 7)),
                         reads=[r_wg, r_hTb], writes=[RP[b0 + 3]], add=(k > 0))
                S.op("act", lambda e: e.activation(sga[i][:], PB[b0 + 2][:], AF.Sigmoid, bias=bgt[:, fc:fc + 1]),
                     reads=[RP[b0 + 2], r_bg], writes=[r_sga[i]])
                S.op("act", lambda e: e.activation(sgb[i][:], PB[b0 + 3][:], AF.Sigmoid, bias=bgt[:, 8 + fc:9 + fc]),
                     reads=[RP[b0 + 3], r_bg], writes=[r_sgb[i]])
                S.op("dve", lambda e: e.tensor_tensor(z1[i][:], PB[b0][:], sga[i][:], ALU.mult),
                     reads=[RP[b0], r_sga[i]], writes=[r_z1[i]])
                S.op("dve", lambda e: e.tensor_tensor(z2[i][:], PB[b0 + 1][:], sgb[i][:], ALU.mult),
                     reads=[RP[b0 + 1], r_sgb[i]], writes=[r_z2[i]])
                S.op("pool", lambda e: e.tensor_tensor(zT[:, fc, :], z1[i][:], z2[i][:], ALU.add),
                     reads=[r_z1[i], r_z2[i]], writes=[r_zT[fc]])
            n = 0
            for j in range(4):
                for half in range(2):
                    b = n % 8
                    n += 1
                    for k in range(8):
                        S.op("pe", lambda e: e.matmul(PB[b][:], zT[:, k, j * 128:(j + 1) * 128],
                                                      wo[:, k, half * 512:(half + 1) * 512], start=(k == 0), stop=(k == 7)),
                             reads=[r_zT[k], r_wo], writes=[RP[b]], add=(k > 0))
                    S.op("dve", lambda e: e.tensor_tensor(xm[:, j, half * 512:(half + 1) * 512], PB[b][:],
                                                          xt[:, j, half * 512:(half + 1) * 512], ALU.add),
                         reads=[RP[b], r_xt], writes=[r_xm], add=(n > 1))
            S.dma("pool", xm_d[tsl, :].rearrange("(j p) d -> p j d", p=128), xm[:], reads=[r_xm])
        S.barrier()


def PHASE_E(nc, S, PB, RP, G):
    g2, wr, br, wgu_s, wdn_s = G["g2"], G["wr"], G["br"], G["wgu_s"], G["wdn_s"]
    xm_d, out, evac = G["xm_d"], G["out"], G["evac"]
    identf, r_idf = G["identf"], G["r_idf"]
    identb, r_idb = G["identb"], G["r_idb"]
    S.barrier()
    with ExitStack() as st:
        def sb(n, s, d):
            return st.enter_context(nc.sbuf_tensor(n, s, d))
        g2t = sb("g2t", [128, D], F32); r_g2 = Res()
        S.dma("sp", g2t[:], g2.broadcast_to([128, D]), writes=[r_g2])
        wrt = sb("wrt", [128, 8, 36], F32); r_wr = Res()
        S.dma("sp", wrt[:], wr.rearrange("(k p) n -> p k n", p=128), writes=[r_wr])
        brt = sb("brt", [128, 36], F32); r_br = Res()
        S.dma("sp", brt[:], br.broadcast_to([128, 36]), writes=[r_br])
        xm = sb("xmE", [128, 4, D], F32); r_xm = Res()
        junk = sb("junkE", [128, D], F32)
        ss = sb("ssE", [128, 4], F32); r_ss = Res()
        h2 = sb("h2", [128, D], F32); r_h2 = Res()
        h2T32 = sb("h2T32", [128, 8, 128], F32); r_h2T32 = Res()
        h2Tbs = [sb(f"h2Tb{i}", [128, 8, 512], BF16) for i in range(2)]; r_h2Tbs = [Res(), Res()]
        lg = sb("lg", [128, 36], F32); r_lg = Res()
        sm = sb("sm", [128, 64], F32); r_sm = Res()
        comb = sb("comb", [128, 128], F32); r_comb = Res()
        S.op("pool", lambda e: e.memset(comb[:], 0.0), writes=[r_comb])
        cTs = [sb(f"cT{i}", [128, 512], F32) for i in range(2)]; r_cTs = [Res(), Res()]
        cHs = [sb(f"cH{i}", [128, 512], BF16) for i in range(2)]; cLs = [sb(f"cL{i}", [128, 512], BF16) for i in range(2)]
        cD = sb("cD", [128, 128], F32); r_cD = Res()
        bc = [sb(f"bc{i}", [128, 512], F32) for i in range(2)]; r_bc = [Res(), Res()]
        NWG = 4
        wgu = [sb(f"wgu{i}", [128, 8, 512], BF16) for i in range(NWG)]; r_wgu = [Res() for _ in range(NWG)]
        wdn = [sb(f"wdn{i}", [128, 8, D], BF16) for i in range(2)]; r_wdn = [Res() for _ in range(2)]
        heT = [sb(f"heT{i}", [128, 8, 512], BF16) for i in range(2)]; r_heT = [Res(), Res()]
        sg = [sb(f"sgE{i}", [128, 512], F32) for i in range(2)]; r_sg = [Res(), Res()]
        tt = [sb(f"ttE{i}", [128, 512], F32) for i in range(2)]; r_tt = [Res(), Res()]
        yaccs = [sb(f"yacc{i}", [128, 4, D], F32) for i in range(2)]; r_yas = [Res(), Res()]
        st_ = {"nwg": 0}

        def router_gen(tb):
            h2Tb, r_h2Tb = h2Tbs[tb % 2], r_h2Tbs[tb % 2]
            cT, r_cT = cTs[tb % 2], r_cTs[tb % 2]
            cH, cL = cHs[tb % 2], cLs[tb % 2]
            tsl = slice(tb * 512, (tb + 1) * 512)
            S.dma("pool", xm[:], xm_d[tsl, :].rearrange("(j p) d -> p j d", p=128), writes=[r_xm]); yield
            for _ in range(20):
                yield
            for j in range(4):
                S.op("act", lambda e: e.activation(junk[:], xm[:, j, :], AF.Square, accum_out=ss[:, j:j + 1]),
                     reads=[r_xm], writes=[r_ss], add=(j > 0)); yield
            S.op("act", lambda e: e.activation(ss[:], ss[:], AF.Sqrt, bias=1e-6, scale=1.0 / D), reads=[r_ss], writes=[r_ss]); yield
            S.op("dve", lambda e: e.reciprocal(ss[:], ss[:]), reads=[r_ss], writes=[r_ss]); yield
            for _ in range(4):
                yield
            for j in range(4):
                S.op("dve", lambda e: e.scalar_tensor_tensor(h2[:], xm[:, j, :], ss[:, j:j + 1], g2t[:], ALU.mult, ALU.mult),
                     reads=[r_xm, r_ss, r_g2], writes=[r_h2]); yield
                yield
                yield
                for k0 in range(0, 8, 4):
                    for k in range(k0, k0 + 4):
                        S.op("pe", lambda e: e.matmul(PB[7][:, (k - k0) * 128:(k - k0 + 1) * 128], h2[:, k * 128:(k + 1) * 128], identf[:],
                                                      start=True, stop=True),
                             reads=[r_h2, r_idf], writes=[RP[7]], add=(k > k0))
                    yield
                    S.op("act", lambda e: e.copy(h2T32[:, k0:k0 + 4, :], PB[7][:].rearrange("p (k t) -> p k t", t=128)),
                         reads=[RP[7]], writes=[r_h2T32], add=(k0 > 0)); yield
                    S.op("dve", lambda e: e.tensor_copy(h2Tb[:, k0:k0 + 4, j * 128:(j + 1) * 128], PB[7][:].rearrange("p (k t) -> p k t", t=128)),
                         reads=[RP[7]], writes=[r_h2Tb], add=not (j == 0 and k0 == 0)); yield
                yield
                yield
                yield
                for k in range(8):
                    S.op("pe", lambda e: e.matmul(PB[7][:, 0:36], h2T32[:, k, :], wrt[:, k, :], start=(k == 0), stop=(k == 7)),
                         reads=[r_h2T32, r_wr], writes=[RP[7]], add=(k > 0))
                yield
                ops = []
                g = 0
                V = lambda f: ops.append(f)
                V(lambda e, g=g: e.tensor_tensor(lg[:], PB[7][:, 0:36], brt[:], ALU.add))
                V(lambda e, g=g: e.tensor_reduce(sm[:, 0:1], lg[:, 0:4], AX.X, ALU.max))
                V(lambda e, g=g: e.tensor_scalar(sm[:, 4:8], lg[:, 0:4], sm[:, 0:1], None, ALU.is_equal))
                V(lambda e, g=g: e.tensor_scalar(sm[:, 8:12], lg[:, 0:4], sm[:, 0:1], None, ALU.subtract))
                ops.append(("act", lambda e: e.activation(sm[:, 8:12], sm[:, 8:12], AF.Exp, accum_out=sm[:, 1:2])))
                V(lambda e, g=g: e.reciprocal(sm[:, 2:3], sm[:, 1:2]))
                V(lambda e, g=g: e.tensor_scalar(sm[:, 16:24], lg[:, 4:12], sm[:, 4:5], None, ALU.mult))
                for g in range(1, 4):
                    V(lambda e, g=g: e.scalar_tensor_tensor(sm[:, 16:24], lg[:, 4 + 8 * g:12 + 8 * g], sm[:, 4 + g:5 + g],
                                                       sm[:, 16:24], ALU.mult, ALU.add))
                V(lambda e, g=g: e.tensor_reduce(sm[:, 3:4], sm[:, 16:24], AX.X, ALU.max))
                V(lambda e, g=g: e.tensor_scalar(sm[:, 24:32], sm[:, 16:24], sm[:, 3:4], None, ALU.is_equal))
                V(lambda e, g=g: e.scalar_tensor_tensor(sm[:, 32:40], sm[:, 24:32], -1e30, sm[:, 16:24], ALU.mult, ALU.add))
                V(lambda e, g=g: e.tensor_reduce(sm[:, 12:13], sm[:, 32:40], AX.X, ALU.max))
                V(lambda e, g=g: e.tensor_scalar(sm[:, 40:48], sm[:, 32:40], sm[:, 12:13], None, ALU.is_equal))
                V(lambda e, g=g: e.tensor_tensor(sm[:, 13:14], sm[:, 12:13], sm[:, 3:4], ALU.subtract))
                ops.append(("act", lambda e: e.activation(sm[:, 13:14], sm[:, 13:14], AF.Exp)))
                V(lambda e, g=g: e.tensor_scalar(sm[:, 14:15], sm[:, 13:14], 1.0, None, ALU.add))
                V(lambda e, g=g: e.reciprocal(sm[:, 14:15], sm[:, 14:15]))
                V(lambda e, g=g: e.tensor_tensor(sm[:, 15:16], sm[:, 14:15], sm[:, 13:14], ALU.mult))
                V(lambda e, g=g: e.tensor_tensor(sm[:, 14:15], sm[:, 14:15], sm[:, 2:3], ALU.mult))
                V(lambda e, g=g: e.tensor_tensor(sm[:, 15:16], sm[:, 15:16], sm[:, 2:3], ALU.mult))
                V(lambda e, g=g: e.tensor_scalar(sm[:, 48:56], sm[:, 24:32], sm[:, 14:15], None, ALU.mult))
                V(lambda e, g=g: e.scalar_tensor_tensor(sm[:, 48:56], sm[:, 40:48], sm[:, 15:16], sm[:, 48:56], ALU.mult, ALU.add))
                for g in range(4):
                    V(lambda e, g=g: e.tensor_scalar(comb[:, 8 * g:8 * g + 8], sm[:, 48:56], sm[:, 4 + g:5 + g], None, ALU.mult))
                for f in ops:
                    if isinstance(f, tuple):
                        S.op("act", f[1], reads=[r_sm], writes=[r_sm])
                    else:
                        S.op("dve", f, reads=[r_lg, r_sm, r_comb, r_br, RP[7]], writes=[r_lg, r_sm, r_comb])
                    yield
                yield
                yield
                yield
                S.op("pe", lambda e: e.matmul(PB[7][:, 0:128], comb[:], identf[:], start=True, stop=True),
                     reads=[r_comb, r_idf], writes=[RP[7]]); yield
                S.op("act", lambda e: e.copy(cT[:, j * 128:(j + 1) * 128], PB[7][:, 0:128]), reads=[RP[7]], writes=[r_cT],
                     add=(j > 0)); yield
                jsl = slice(j * 128, (j + 1) * 128)
                S.op("dve", lambda e: e.tensor_copy(cH[:, jsl], cT[:, jsl]), reads=[r_cT], writes=[r_cT], add=True); yield
                S.op("dve", lambda e: e.tensor_tensor(cD[:], cT[:, jsl], cH[:, jsl], ALU.subtract), reads=[r_cT], writes=[r_cD]); yield
                S.op("dve", lambda e: e.tensor_copy(cL[:, jsl], cD[:]), reads=[r_cD], writes=[r_cT], add=True); yield

        def expert_group(tb, eg, gen=None):
            h2Tb, r_h2Tb = h2Tbs[tb % 2], r_h2Tbs[tb % 2]
            cT, r_cT = cTs[tb % 2], r_cTs[tb % 2]
            cH, cL = cHs[tb % 2], cLs[tb % 2]
            hi = eg % 2
            dn = wdn[eg % 2]
            S.dma("sp", dn[:].rearrange("p (e c) n -> p e c n", c=2),
                  wdn_s[eg * 4:(eg + 1) * 4].rearrange("e p c n -> p e c n"), writes=[r_wdn[eg % 2]])
            for el in range(4):
                ex = eg * 4 + el
                wi = st_["nwg"] % NWG
                st_["nwg"] += 1
                S.dma("sp", wgu[wi][:], wgu_s[ex], writes=[r_wgu[wi]])
                bi = ex % 2
                S.op("pe", lambda e: e.matmul(PB[6][:], identb[:, ex:ex + 1].broadcast_to([128, 128]), cH[:], start=True, stop=False),
                     reads=[r_idb, r_cT], writes=[RP[6]])
                S.op("pe", lambda e: e.matmul(PB[6][:], identb[:, ex:ex + 1].broadcast_to([128, 128]), cL[:], start=False, stop=True),
                     reads=[r_idb, r_cT], writes=[RP[6]], add=True)
                S.op("act", lambda e: e.copy(bc[bi][:], PB[6][:]), reads=[RP[6]], writes=[r_bc[bi]])
                for c in range(2):
                    pg, pu = (0, 1) if (c == 0) else (2, 3)
                    for k in range(8):
                        S.op("pe", lambda e: e.matmul(PB[pg][:], wgu[wi][:, k, c * 128:(c + 1) * 128], h2Tb[:, k, :],
                                                      start=(k == 0), stop=(k == 7)),
                             reads=[r_wgu[wi], r_h2Tb], writes=[RP[pg]], add=(k > 0))
                    for k in range(8):
                        S.op("pe", lambda e: e.matmul(PB[pu][:], wgu[wi][:, k, 256 + c * 128:256 + (c + 1) * 128],
                                                      h2Tb[:, k, :], start=(k == 0), stop=(k == 7)),
                             reads=[r_wgu[wi], r_h2Tb], writes=[RP[pu]], add=(k > 0))
                    S.op("act", lambda e: e.activation(sg[c][:], PB[pg][:], AF.Silu), reads=[RP[pg]], writes=[r_sg[c]])
                    S.op("dve", lambda e: e.tensor_tensor(tt[c][:], PB[pu][:], sg[c][:], ALU.mult),
                         reads=[RP[pu], r_sg[c]], writes=[r_tt[c]])
                    S.op("pool", lambda e: e.tensor_tensor(heT[hi][:, el * 2 + c, :], tt[c][:], bc[bi][:], ALU.mult),
                         reads=[r_tt[c], r_bc[bi]], writes=[r_heT[hi]], add=(el * 2 + c > 0))
                    if gen is not None:
                        for _ in range(10):
                            next(gen, None)
            n = 0
            for j in range(4):
                for half in range(2):
                    b = 4 + n % 2
                    n += 1
                    for kk in range(8):
                        S.op("pe", lambda e: e.matmul(PB[b][:], heT[hi][:, kk, j * 128:(j + 1) * 128],
                                                      dn[:, kk, half * 512:(half + 1) * 512],
                                                      start=(kk == 0), stop=(kk == 7)),
                             reads=[r_heT[hi], r_wdn[eg % 2]], writes=[RP[b]], add=(kk > 0))
                    hsl = slice(half * 512, (half + 1) * 512)
                    yn, r_yn = yaccs[eg % 2], r_yas[eg % 2]
                    yo_, r_yo_ = yaccs[(eg + 1) % 2], r_yas[(eg + 1) % 2]
                    if eg == 0:
                        S.op("dve", lambda e: e.tensor_tensor(yn[:, j, hsl], PB[b][:], xm[:, j, hsl], ALU.add),
                             reads=[RP[b], r_xm], writes=[r_yn], add=(n > 1))
                    else:
                        S.op("dve", lambda e: e.tensor_tensor(yn[:, j, hsl], PB[b][:], yo_[:, j, hsl], ALU.add),
                             reads=[RP[b], r_yo_], writes=[r_yn], add=(n > 1))

        for _ in router_gen(0):
            pass
        for tb in range(8):
            tsl = slice(tb * 512, (tb + 1) * 512)
            gen = None
            for eg in range(8):
                expert_group(tb, eg, gen)
                if eg == 0 and tb + 1 < 8:
                    gen = router_gen(tb + 1)
            if gen is not None:
                for _ in gen:
                    pass
            S.dma("pool", out[tsl, :].rearrange("(j p) d -> p j d", p=128), yaccs[1][:], reads=[r_yas[1]])
        S.barrier()


def PHASE_C(nc, S, PB, RP, G):
    p_d, yb_d, qkg, btab, nmask, bonesd, evac = G["p_d"], G["yb_d"], G["qkg"], G["btab"], G["nmask"], G["bonesd"], G["evac"]
    identb, r_idb = G["identb"], G["r_idb"]
    with ExitStack() as st:
        def sb(n, s, d):
            return st.enter_context(nc.sbuf_tensor(n, s, d))
        BT = sb("BT", [128, 8, 14, 64], F32); r_BT = Res()
        mk = sb("mk", [128, 64], F32); r_mk = Res()
        gk = sb("gk", [128, 2], F32); r_gk = Res()
        bones = sb("bonesC", [128, 128], F32); r_bones = Res()
        S.dma("sp", BT[:], btab, writes=[r_BT])
        S.dma("sp", mk[:], nmask, writes=[r_mk])
        S.dma("sp", gk[:], qkg, writes=[r_gk])
        S.dma("sp", bones[:], bonesd, writes=[r_bones])
        for h in range(8):
            for pp in range(14):
                S.op("pool", lambda e: e.tensor_tensor(BT[:, h, pp, :], BT[:, h, pp, :], mk[:], ALU.add),
                     reads=[r_BT, r_mk], writes=[r_BT])
        qf = sb("qf", [128, T], F32); r_qf = Res()
        kf = sb("kf", [128, T], F32); r_kf = Res()
        vf = sb("vf", [128, T], F32); r_vf = Res()
        sq = sb("sqC", [128, T], F32); r_sq = Res()
        rq = sb("rqC", [128, T], F32); r_rq = Res()
        qn = sb("qn", [128, T], BF16); r_qn = Res()
        kn = sb("kn", [128, T], BF16); r_kn = Res()
        vb = sb("vb", [128, T], BF16); r_vb = Res()
        Vtm = [sb(f"Vtm{a}", [128, 16, 2, 65], BF16) for a in range(2)]; r_V = [Res(), Res()]
        for a in range(2):
            S.op("pool", lambda e: e.memset(Vtm[a][:], 1.0), writes=[r_V[a]])
        scs = [sb(f"scs{i}", [128, 4, 64], F32) for i in range(4)]; r_scs = [Res() for _ in range(4)]
        Eb = [sb(f"Eb{i}", [128, 4, 64], BF16) for i in range(4)]; r_E = [Res() for _ in range(4)]
        rden = [sb(f"rden{i}", [64, 1], F32) for i in range(2)]; r_rden = [Res(), Res()]
        otm = sb("otm", [64, 32, 128], BF16); r_otm = Res()
        ybT = sb("ybT", [128, T], BF16); r_ybT = Res()
        for s_ in range(2):
            t0 = s_ * T
            for hp in range(4):
                S.dma("sp", qf[:], p_d[1920 + hp * 128:1920 + (hp + 1) * 128, t0:t0 + T], writes=[r_qf])
                S.dma("sp", kf[:], p_d[2432 + hp * 128:2432 + (hp + 1) * 128, t0:t0 + T], writes=[r_kf])
                S.dma("sp", vf[:], p_d[2944 + hp * 128:2944 + (hp + 1) * 128, t0:t0 + T], writes=[r_vf])
                for (src, r_src, dst, r_dst, gi, sc, bi) in ((qf, r_qf, qn, r_qn, 0, 1.0, 64e-6), (kf, r_kf, kn, r_kn, 1, 1.0 / 64, 1e-6)):
                    S.op("act", lambda e: e.activation(sq[:], src[:], AF.Square), reads=[r_src], writes=[r_sq])
                    for c in range(4):
                        b = c % 2
                        S.op("pe", lambda e: e.matmul(PB[b][:], bones[:], sq[:, c * 512:(c + 1) * 512], start=True, stop=True),
                             reads=[r_bones, r_sq], writes=[RP[b]])
                        S.op("act", lambda e: e.activation(rq[:, c * 512:(c + 1) * 512], PB[b][:], AF.Sqrt, bias=bi, scale=sc),
                             reads=[RP[b]], writes=[r_rq], add=(c > 0))
                    S.op("dve", lambda e: e.reciprocal(rq[:], rq[:]), reads=[r_rq], writes=[r_rq])
                    S.op("dve", lambda e: e.scalar_tensor_tensor(dst[:], src[:], gk[:, gi:gi + 1], rq[:], ALU.mult, ALU.mult),
                         reads=[r_src, r_gk, r_rq], writes=[r_dst])
                S.op("act", lambda e: e.copy(vb[:], vf[:]), reads=[r_vf], writes=[r_vb])
                for a in range(2):
                    nb = 16 - a
                    for b0 in range(0, nb, 4):
                        n4 = min(4, nb - b0)
                        pb = 2 + (b0 // 4) % 2
                        for q in range(n4):
                            tk = a * 64 + (b0 + q) * 128
                            S.op("pe", lambda e: e.matmul(PB[pb][:, q * 128:(q + 1) * 128], vb[:, tk:tk + 128], identb[:],
                                                          start=True, stop=True),
                                 reads=[r_vb, r_idb], writes=[RP[pb]], add=(q > 0))
                        evac(Vtm[a][:, b0:b0 + n4, :, 0:64],
                             PB[pb][:, 0:n4 * 128].rearrange("p (b h d) -> p b h d", h=2, d=64),
                             [RP[pb]], [r_V[a]], add=(b0 > 0))
                units = [(hl, i) for hl in range(2) for i in range(32)]

                def na_stage1(n):
                    hl, i = units[n]
                    h = hp * 2 + hl
                    hs = slice(hl * 64, (hl + 1) * 64)
                    start = min(max(i - 4, 0), 24)
                    pp0 = start - i + 7
                    u = n % 4
                    ps = (0, 1, 4, 5)[u]
                    for c in range(4):
                        kt = start * 64 + c * 128
                        S.op("pe", lambda e: e.matmul(PB[ps][:, c * 64:(c + 1) * 64], kn[hs, kt:kt + 128],
                                                      qn[hs, i * 64:(i + 1) * 64], start=True, stop=True),
                             reads=[r_kn, r_qn], writes=[RP[ps]], add=(c > 0))
                    S.op("dve", lambda e: e.tensor_tensor(scs[u][:], PB[ps][:, 0:256].rearrange("p (c q) -> p c q", q=64),
                                                          BT[:, h, pp0:pp0 + 7:2, :], ALU.add),
                         reads=[RP[ps], r_BT], writes=[r_scs[u]])
                    S.op("act", lambda e: e.activation(Eb[u][:], scs[u][:], AF.Exp), reads=[r_scs[u]], writes=[r_E[u]])

                def na_stage2(n):
                    hl, i = units[n]
                    hs = slice(hl * 64, (hl + 1) * 64)
                    start = min(max(i - 4, 0), 24)
                    a = start % 2
                    blk0 = start // 2
                    u = n % 4
                    v = n % 2
                    po = 6 + v
                    for c in range(4):
                        S.op("pe", lambda e: e.matmul(PB[po][0:64, 0:65], Eb[u][:, c, :], Vtm[a][:, blk0 + c, hl, :],
                                                      start=(c == 0), stop=(c == 3)),
                             reads=[r_E[u], r_V[a]], writes=[RP[po]], add=(c > 0))
                    S.op("dve", lambda e: e.reciprocal(rden[v][:], PB[po][0:64, 64:65]), reads=[RP[po]], writes=[r_rden[v]])
                    S.op("dve", lambda e: e.tensor_scalar(otm[:, i, hs], PB[po][0:64, 0:64], rden[v][:], None, ALU.mult),
                         reads=[RP[po], r_rden[v]], writes=[r_otm], add=(n > 0))

                na_stage1(0)
                na_stage1(1)
                for n in range(0, 64, 2):
                    if n + 2 < 64:
                        na_stage1(n + 2)
                        na_stage1(n + 3)
                    na_stage2(n)
                    na_stage2(n + 1)
                for i0 in range(0, 32, 8):
                    pb = (i0 // 8) % 2
                    for q in range(8):
                        S.op("pe", lambda e: e.matmul(PB[pb][:, q * 64:(q + 1) * 64], otm[:, i0 + q, :], identb[0:64, 0:64],
                                                      start=True, stop=True),
                             reads=[r_otm, r_idb], writes=[RP[pb]], add=(q > 0))
                    evac(ybT[:, i0 * 64:(i0 + 8) * 64], PB[pb][:], [RP[pb]], [r_ybT], add=(i0 > 0))
                S.dma("pool", yb_d[hp * 128:(hp + 1) * 128, t0:t0 + T], ybT[:], reads=[r_ybT])
        S.barrier()


def _prep(inputs):
    f = lambda a: np.ascontiguousarray(np.asarray(a, dtype=np.float32))
    I = {k: np.asarray(v) for k, v in inputs.items()}
    com = {}
    com["g1"] = f(I["norm1_g"][0][None, :])
    com["g2"] = f(I["norm2_g"][0][None, :])
    com["w_in"] = f(I["w_in"][0])
    com["mus"] = f(np.stack([I["mu_prev"][0].reshape(15, 128).T, I["mu_next"][0].reshape(15, 128).T], axis=1))
    v9 = np.stack([I["w0_f"][0], I["w0_b"][0], I["a0_f"][0], I["a0_b"][0], I["k_k"][0], I["k_a"][0],
                   I["r_k"][0].reshape(512), I["lnx_g"][0], I["lnx_b"][0]])
    com["vec"] = f(v9.reshape(9, 4, 128).transpose(2, 0, 1))
    com["lora"] = f(np.stack([np.concatenate([I["w_up_f"][0], I["w_up_b"][0]], 0),
                              np.concatenate([I["a_up_f"][0], I["a_up_b"][0]], 0), I["g_up"][0]], axis=1))
    com["qkg"] = f(np.stack([np.tile(I["q_gain"][0], 2), np.tile(I["k_gain"][0], 2)], axis=1))
    rpb = I["rpb"][0]
    kq = np.clip(np.arange(64)[:, None] - np.arange(64)[None, :] + 15, 0, 30)
    bt = np.zeros((2, 64, 8, 14, 64), np.float32)
    for r2 in range(2):
        for pp in range(14):
            bt[r2, :, :, pp, :] = rpb[:, pp + r2, :][:, kq].transpose(1, 0, 2)
    com["btab"] = f(bt.reshape(128, 8, 14, 64))
    cols = np.arange(64)
    cs = np.clip(cols - 8, 0, 48)
    cm = (cols[None, :] >= cs[:, None]) & (cols[None, :] < cs[:, None] + 16)
    m = np.where(cm.T, 0.0, NEG).astype(np.float32)
    com["nmask"] = f(np.concatenate([m, m], 0))
    com["bg"] = f(I["b_gate"][0].reshape(16, 128).T)
    com["w_oa"] = f(I["w_o_rwkv"][0])
    com["w_ob"] = f(I["w_o_na"][0])
    com["w_out"] = f(I["w_out"][0])
    com["wr"] = f(np.concatenate([I["w_router_group"][0], I["w_router_expert"][0]], 1))
    com["br"] = f(np.concatenate([I["b_router_group"][0], I["b_router_expert"][0]])[None, :])
    com["wge"] = f(I["w_gate_e"][0])
    com["wue"] = f(I["w_up_e"][0])
    com["wde"] = f(I["w_down_e"][0])
    com["ident"] = np.eye(128, dtype=np.float32)
    bo = np.zeros((128, 128), np.float32); bo[:64, :64] = 1; bo[64:, 64:] = 1
    com["bones"] = bo
    s_i = np.arange(64)[:, None]; t_i = np.arange(64)[None, :]
    rm = np.zeros((2, 64, 4, 2, 64), np.float32)
    for d in range(2):
        st_ = (s_i < t_i) if d == 0 else (s_i > t_i)
        inc = (s_i <= t_i) if d == 0 else (s_i >= t_i)
        rm[:, :, 0, d, :] = st_
        rm[:, :, 1, d, :] = inc
        rm[:, :, 2, d, :] = st_.T
        rm[:, :, 3, d, :] = 1.0
    rm = rm.reshape(128, 4, 2, 64)
    com["rmask"] = rm
    sc = np.ones((128, T), np.float32); sc[:, ::64] = 0
    com["scanm"] = sc
    xs = f(I["x"]).reshape(NCORES, NT, D)
    return com, xs


_CACHE = {}


def kernel(**inputs):
    com, xs = _prep(inputs)
    if "nc" not in _CACHE:
        _CACHE["nc"] = build2()
    nc = _CACHE["nc"]
    in_maps = [dict(com, x=xs[c]) for c in range(NCORES)]
    res = run_bass_kernel_spmd(nc, in_maps, core_ids=list(range(NCORES)))
    outs = [np.asarray(res.results[c]["out"], dtype=np.float32) for c in range(NCORES)]
    return np.stack(outs).reshape(16, T, D)
```
